# Optimizing a Trainium2 kernel written in Bass

```python
import jax, jax.numpy as jnp
from jax import lax
import numpy as np

D_MODEL = 1024
BATCH = 4
SEQ = 8192
DEPTH = 4

D_MIX = D_MODEL
N_HEADS_ATT = 8
HD_ATT = 64
D_ATT = N_HEADS_ATT * HD_ATT
DILATED_PATTERNS = ((128, 1), (512, 4), (2048, 16))
N_HEADS_ML = 4
HD_ML = 128
D_ML = N_HEADS_ML * HD_ML
CONV_W = 4
CHUNK = 64
N_GROUPS = 4
EXPERTS_PER_GROUP = 8
N_EXPERTS = N_GROUPS * EXPERTS_PER_GROUP
TOP_K = 2
D_EXPERT = 512
MOE_BLOCK = 128
EPS = 1e-6
SPLIT_SIZES = (D_ATT, D_ATT, D_ATT, D_ML, D_ML, D_ML, D_ML, N_HEADS_ML, N_HEADS_ML)
D_IN = sum(SPLIT_SIZES)
SPLIT_POINTS = tuple(int(v) for v in np.cumsum(SPLIT_SIZES)[:-1])

kernel_name = 'hymba_dilated_mlstm_hmoe_trunk'


def _rmsnorm(x, g):
    xf = x.astype(jnp.float32)
    y = xf * lax.rsqrt(jnp.mean(xf * xf, axis=-1, keepdims=True) + EPS)
    return (y * g.astype(jnp.float32)).astype(x.dtype)


def _dilated_window_branch(q, k, v, window, dilation):
    b, h, s, hd = q.shape
    band = window // dilation
    span = band * dilation
    s_pad = -(-s // span) * span
    length = s_pad // dilation
    n_blk = length // band

    def to_blocks(t):
        t = jnp.pad(t, ((0, 0), (0, 0), (0, s_pad - s), (0, 0)))
        t = t.reshape(b, h, length, dilation, hd).transpose(0, 1, 3, 2, 4)
        return t.reshape(b, h, dilation, n_blk, band, hd)

    def with_prev(t):
        prev = jnp.pad(t, ((0, 0), (0, 0), (0, 0), (1, 0), (0, 0), (0, 0)))[:, :, :, :-1]
        return jnp.concatenate([prev, t], axis=4)

    qb = to_blocks(q)
    kc = with_prev(to_blocks(k))
    vc = with_prev(to_blocks(v))
    scores = jnp.einsum('bhrnqd,bhrnkd->bhrnqk', qb, kc)
    q_idx = band + jnp.arange(band)
    k_idx = jnp.arange(2 * band)
    dist = q_idx[:, None] - k_idx[None, :]
    in_band = (dist >= 0) & (dist <= band)
    has_prev = (jnp.arange(n_blk) > 0)[:, None, None] | (k_idx >= band)[None, None, :]
    mask = in_band[None] & has_prev
    scores = jnp.where(mask, scores, -jnp.inf)
    m = scores.max(axis=-1)
    p = jnp.exp(scores - m[..., None])
    l = p.sum(axis=-1)
    o = jnp.einsum('bhrnqk,bhrnkd->bhrnqd', p, vc)

    def from_blocks(t):
        t = t.reshape(b, h, dilation, length, -1).transpose(0, 1, 3, 2, 4).reshape(b, h, s_pad, -1)
        return t[:, :, :s]

    return from_blocks(o), from_blocks(m[..., None])[..., 0], from_blocks(l[..., None])[..., 0]


def _dilated_attention(q, k, v):
    outs = [_dilated_window_branch(q, k, v, w, d) for (w, d) in DILATED_PATTERNS]
    m_all = jnp.maximum(jnp.maximum(outs[0][1], outs[1][1]), outs[2][1])
    num = jnp.zeros_like(q)
    den = jnp.zeros_like(m_all)
    for o_i, m_i, l_i in outs:
        a_i = jnp.exp(m_i - m_all)
        num = num + a_i[..., None] * o_i
        den = den + a_i * l_i
    return num / den[..., None]


def _mlstm(q, k, v, i_pre, log_f):
    b, h, s, dk = q.shape
    dv = v.shape[-1]
    nc = s // CHUNK

    def chunks(t):
        return jnp.moveaxis(t.reshape((b, h, nc, CHUNK) + t.shape[3:]), 2, 0)

    causal = jnp.tril(jnp.ones((CHUNK, CHUNK), dtype=bool))

    def step(carry, inp):
        c_st, n_st, m_st = carry
        qc, kc, vc, ic, fc = inp
        bcum = jnp.cumsum(fc, axis=-1)
        d_log = bcum[..., :, None] - bcum[..., None, :] + ic[..., None, :]
        d_log = jnp.where(causal, d_log, -jnp.inf)
        m_inter = bcum + m_st[..., None]
        m_comb = jnp.maximum(m_inter, d_log.max(axis=-1))
        w_intra = jnp.exp(d_log - m_comb[..., None])
        w_inter = jnp.exp(m_inter - m_comb)
        qk = jnp.einsum('bhtd,bhsd->bhts', qc, kc) * w_intra
        num = jnp.einsum('bhts,bhsv->bhtv', qk, vc) + w_inter[..., None] * jnp.einsum('bhtd,bhdv->bhtv', qc, c_st)
        den = qk.sum(axis=-1) + w_inter * jnp.einsum('bhtd,bhd->bht', qc, n_st)
        h_out = num / jnp.maximum(jnp.abs(den), jnp.exp(-m_comb))[..., None]
        b_last = bcum[..., -1]
        decay_s = b_last[..., None] - bcum + ic
        m_new = jnp.maximum(b_last + m_st, decay_s.max(axis=-1))
        w_s = jnp.exp(decay_s - m_new[..., None])
        w_c = jnp.exp(b_last + m_st - m_new)
        c_new = w_c[..., None, None] * c_st + jnp.einsum('bhs,bhsd,bhsv->bhdv', w_s, kc, vc)
        n_new = w_c[..., None] * n_st + jnp.einsum('bhs,bhsd->bhd', w_s, kc)
        return (c_new, n_new, m_new), h_out

    init = (jnp.zeros((b, h, dk, dv), jnp.float32), jnp.zeros((b, h, dk), jnp.float32), jnp.zeros((b, h), jnp.float32))
    xs = (chunks(q), chunks(k), chunks(v), chunks(i_pre), chunks(log_f))
    _, hs = lax.scan(step, init, xs)
    return jnp.moveaxis(hs, 0, 2).reshape(b, h, s, dv)


def _mixer(xn, w_in, b_igate, b_fgate, conv_w, g_q, g_k, w_out):
    b, s, _ = xn.shape
    proj = xn @ w_in
    qa, ka, va, qm, km, vm, om, ig, fg = jnp.split(proj, SPLIT_POINTS, axis=-1)
    f32 = jnp.float32

    def heads(t, nh, hd):
        return t.astype(f32).reshape(b, s, nh, hd).transpose(0, 2, 1, 3)
    qa = _rmsnorm(heads(qa, N_HEADS_ATT, HD_ATT), g_q) * (HD_ATT ** -0.5)
    ka = _rmsnorm(heads(ka, N_HEADS_ATT, HD_ATT), g_k)
    va = heads(va, N_HEADS_ATT, HD_ATT)
    y_att = _dilated_attention(qa, ka, va).transpose(0, 2, 1, 3).reshape(b, s, D_ATT)

    qk_in = jnp.concatenate([qm, km], axis=-1).astype(f32)
    qk_conv = lax.conv_general_dilated(
        qk_in, conv_w.astype(f32)[:, None, :], window_strides=(1,), padding=[(CONV_W - 1, 0)],
        dimension_numbers=('NWC', 'WIO', 'NWC'), feature_group_count=2 * D_ML)
    qk_conv = jax.nn.silu(qk_conv)
    qm_c, km_c = qk_conv[..., :D_ML], qk_conv[..., D_ML:]
    qml = heads(qm_c, N_HEADS_ML, HD_ML)
    kml = heads(km_c, N_HEADS_ML, HD_ML) * (HD_ML ** -0.5)
    vml = heads(vm, N_HEADS_ML, HD_ML)
    i_pre = (ig.astype(f32) + b_igate.astype(f32)).transpose(0, 2, 1)
    log_f = jax.nn.log_sigmoid(fg.astype(f32) + b_fgate.astype(f32)).transpose(0, 2, 1)
    h_ml = _mlstm(qml, kml, vml, i_pre, log_f).transpose(0, 2, 1, 3).reshape(b, s, D_ML)
    y_ml = jax.nn.sigmoid(om.astype(f32)) * h_ml

    y = jnp.concatenate([y_att, y_ml], axis=-1).astype(xn.dtype)
    return y @ w_out


def _hier_moe(xn, w_group, b_group, w_expert_router, b_expert_router, w1, w3, w2):
    b, s, d = xn.shape
    t = b * s
    xf = xn.reshape(t, d)
    g_logits = (xf @ w_group).astype(jnp.float32) + b_group.astype(jnp.float32)
    g_prob = jax.nn.softmax(g_logits, axis=-1)
    g_sel = jnp.argmax(g_logits, axis=-1)
    g_gate = jnp.take_along_axis(g_prob, g_sel[:, None], axis=-1)
    e_logits = ((xf @ w_expert_router).astype(jnp.float32) + b_expert_router.astype(jnp.float32))
    e_logits = e_logits.reshape(t, N_GROUPS, EXPERTS_PER_GROUP)
    e_logits = jnp.take_along_axis(e_logits, g_sel[:, None, None], axis=1)[:, 0]
    top_vals, top_idx = lax.top_k(e_logits, TOP_K)
    gates = g_gate * jax.nn.softmax(top_vals, axis=-1)

    n_assign = t * TOP_K
    expert_id = (g_sel[:, None] * EXPERTS_PER_GROUP + top_idx).reshape(-1)
    token_id = jnp.repeat(jnp.arange(t, dtype=jnp.int32), TOP_K)
    gate_flat = gates.reshape(-1)
    order = jnp.argsort(expert_id)
    e_sorted = expert_id[order]
    tok_sorted = token_id[order]
    gate_sorted = gate_flat[order]
    counts = jnp.bincount(expert_id, length=N_EXPERTS)
    padded = (counts + MOE_BLOCK - 1) // MOE_BLOCK * MOE_BLOCK
    start = jnp.cumsum(counts) - counts
    pend = jnp.cumsum(padded)
    pstart = pend - padded
    dest = pstart[e_sorted] + (jnp.arange(n_assign) - start[e_sorted])
    n_blk = -(-n_assign // MOE_BLOCK) + N_EXPERTS
    cap = n_blk * MOE_BLOCK
    buf_tok = jnp.zeros((cap,), jnp.int32).at[dest].set(tok_sorted)
    buf_gate = jnp.zeros((cap,), jnp.float32).at[dest].set(gate_sorted)
    blk_expert = jnp.minimum(jnp.searchsorted(pend, jnp.arange(n_blk) * MOE_BLOCK, side='right'), N_EXPERTS - 1)
    xb = xf[buf_tok].reshape(n_blk, MOE_BLOCK, d)

    def expert_block(args):
        xblk, e = args
        hdn = jax.nn.silu(xblk @ w1[e]) * (xblk @ w3[e])
        return hdn @ w2[e]

    yb = lax.map(expert_block, (xb, blk_expert)).reshape(cap, d)
    y = jnp.zeros_like(xf).at[buf_tok].add(yb * buf_gate[:, None].astype(yb.dtype))
    return y.reshape(b, s, d)


def setup_inputs(seed: int = 0) -> dict:
    key = jax.random.key(seed)
    ks = jax.random.split(key, 20)
    f32 = jnp.float32
    nrm = lambda k, shape, scale: jax.random.normal(k, shape, f32) * scale
    x = nrm(ks[0], (BATCH, SEQ, D_MODEL), 1.0)
    g_norm_mix = 1.0 + nrm(ks[1], (DEPTH, D_MODEL), 0.05)
    w_in = nrm(ks[2], (DEPTH, D_MODEL, D_IN), D_MODEL ** -0.5)
    b_igate = nrm(ks[3], (DEPTH, N_HEADS_ML), 0.1)
    b_fgate = jnp.linspace(3.0, 6.0, N_HEADS_ML, dtype=f32)[None, :] + nrm(ks[4], (DEPTH, N_HEADS_ML), 0.1)
    conv_w = nrm(ks[5], (DEPTH, CONV_W, 2 * D_ML), CONV_W ** -0.5)
    g_q = 1.0 + nrm(ks[6], (DEPTH, HD_ATT), 0.05)
    g_k = 1.0 + nrm(ks[7], (DEPTH, HD_ATT), 0.05)
    w_out = nrm(ks[8], (DEPTH, D_MIX, D_MODEL), D_MIX ** -0.5 / np.sqrt(2.0 * DEPTH))
    g_norm_ffn = 1.0 + nrm(ks[9], (DEPTH, D_MODEL), 0.05)
    w_group = nrm(ks[10], (DEPTH, D_MODEL, N_GROUPS), D_MODEL ** -0.5)
    b_group = nrm(ks[11], (DEPTH, N_GROUPS), 0.01)
    w_expert_router = nrm(ks[12], (DEPTH, D_MODEL, N_EXPERTS), D_MODEL ** -0.5)
    b_expert_router = nrm(ks[13], (DEPTH, N_EXPERTS), 0.01)
    w1 = nrm(ks[14], (DEPTH, N_EXPERTS, D_MODEL, D_EXPERT), D_MODEL ** -0.5)
    w3 = nrm(ks[15], (DEPTH, N_EXPERTS, D_MODEL, D_EXPERT), D_MODEL ** -0.5)
    w2 = nrm(ks[16], (DEPTH, N_EXPERTS, D_EXPERT, D_MODEL), D_EXPERT ** -0.5 / np.sqrt(2.0 * DEPTH))
    return {'x': x, 'g_norm_mix': g_norm_mix, 'w_in': w_in, 'b_igate': b_igate, 'b_fgate': b_fgate,
            'conv_w': conv_w, 'g_q': g_q, 'g_k': g_k, 'w_out': w_out, 'g_norm_ffn': g_norm_ffn,
            'w_group': w_group, 'b_group': b_group, 'w_expert_router': w_expert_router,
            'b_expert_router': b_expert_router, 'w1': w1, 'w3': w3, 'w2': w2}


def reference(x, g_norm_mix, w_in, b_igate, b_fgate, conv_w, g_q, g_k, w_out, g_norm_ffn,
              w_group, b_group, w_expert_router, b_expert_router, w1, w3, w2):
    for layer in range(DEPTH):
        h = _rmsnorm(x, g_norm_mix[layer])
        x = x + _mixer(h, w_in[layer], b_igate[layer], b_fgate[layer], conv_w[layer],
                       g_q[layer], g_k[layer], w_out[layer])
        h = _rmsnorm(x, g_norm_ffn[layer])
        x = x + _hier_moe(h, w_group[layer], b_group[layer], w_expert_router[layer], b_expert_router[layer],
                          w1[layer], w3[layer], w2[layer])
    return x
```

```python
import math
from contextlib import ExitStack
import numpy as np
import concourse.bass as bass
import concourse.mybir as mybir
from concourse.bass_utils import run_bass_kernel_spmd

F32 = mybir.dt.float32
BF16 = mybir.dt.bfloat16
I32 = mybir.dt.int32
U32 = mybir.dt.uint32
AF = mybir.ActivationFunctionType
ALU = mybir.AluOpType
AX = mybir.AxisListType

D = 1024
DIN = 3592
NEXP = 32
DEXP = 512
EPS = 1e-6
SEM_LIM = 30000


class Sched:
    def __init__(self, nc):
        self.nc = nc
        self.eng = {'pe': nc.tensor, 'act': nc.scalar, 'dve': nc.vector, 'pool': nc.gpsimd, 'sp': nc.sync}
        self.cnt = {e: 0 for e in self.eng}
        self.sems = {e: [] for e in self.eng}
        self.seen = {e: {} for e in self.eng}
        self.lastw = {}
        self.readers = {}
        self.dma_sems = {}
        self.dma_rr = {}
        self.nwaits = 0
        self.ninst = 0

    def _sem(self, e, n):
        k = (n - 1) // SEM_LIM
        while len(self.sems[e]) <= k:
            self.sems[e].append(self.nc.alloc_semaphore(f"s_{e}_{len(self.sems[e])}"))
        return self.sems[e][k], (n - 1) % SEM_LIM + 1

    def _wait(self, e, evs):
        need = {}
        for ev in evs:
            if ev is None:
                continue
            if ev[0] == 'e':
                _, e2, n = ev
                if e2 == 'pe' and e == 'pe':
                    continue
                key = ('e', e2)
            else:
                _, si, n = ev
                key = ('d', si)
            if n > need.get(key, 0):
                need[key] = n
        for key, n in need.items():
            if self.seen[e].get(key, 0) >= n:
                continue
            if key[0] == 'e':
                sem, val = self._sem(key[1], n)
            else:
                sem, val = self.dma_sem_handles[key[1]], n
            self.eng[e].wait_ge(sem, val)
            self.nwaits += 1
            self.seen[e][key] = n

    def _deps(self, r, w):
        evs = []
        for res in r:
            evs.append(self.lastw.get(res))
        for res in w:
            evs.append(self.lastw.get(res))
            evs.extend(self.readers.get(res, {}).values())
        return evs

    def _commit(self, ev, r, w):
        for res in w:
            self.lastw[res] = ev
            self.readers[res] = {}
        for res in r:
            if res in w:
                continue
            self.readers.setdefault(res, {})[ev[:2]] = ev

    def op(self, e, build, r=(), w=()):
        self._wait(e, self._deps(r, w))
        ins = build(self.eng[e])
        n = self.cnt[e] + 1
        self.cnt[e] = n
        sem, _ = self._sem(e, n)
        ins.then_inc(sem, 1)
        self.ninst += 1
        self._commit(('e', e, n), r, w)

    dma_sem_handles = None

    def dma(self, q, build, r=(), w=()):
        if self.dma_sem_handles is None:
            self.dma_sem_handles = []
            self.dma_val = []
        pool = self.dma_sems.setdefault(q, [])
        if len(pool) < 8:
            si = len(self.dma_sem_handles)
            self.dma_sem_handles.append(self.nc.alloc_semaphore(f"d_{q}_{len(pool)}"))
            self.dma_val.append(0)
            pool.append(si)
            self.dma_rr[q] = len(pool) - 1
        else:
            self.dma_rr[q] = (self.dma_rr[q] + 1) % len(pool)
            si = pool[self.dma_rr[q]]
        evs = self._deps(r, w)
        if self.dma_val[si] > 0:
            evs.append(('d', si, self.dma_val[si]))
        self._wait(q, evs)
        ins = build(self.eng[q])
        self.dma_val[si] += 16
        ins.then_inc(self.dma_sem_handles[si], 16)
        self.ninst += 1
        self._commit(('d', si, self.dma_val[si]), r, w)

    def barrier(self):
        evs = []
        for e in self.eng:
            if self.cnt[e] > 0:
                evs.append(('e', e, self.cnt[e]))
        for si, v in enumerate(self.dma_val or []):
            if v > 0:
                evs.append(('d', si, v))
        for e in self.eng:
            self._wait(e, evs)

    def finish(self, res_list):
        evs = [self.lastw.get(r) for r in res_list]
        for e in self.eng:
            if self.cnt[e] > 0:
                evs.append(('e', e, self.cnt[e]))
        for si, v in enumerate(self.dma_val or []):
            if v > 0:
                evs.append(('d', si, v))
        self._wait('sp', evs)
        self.eng['sp'].nop() if hasattr(self.eng['sp'], 'nop') else None


def host_consts():
    k = np.arange(128)[:, None]
    q = np.arange(128)[None, :]
    c = {}
    c['ident'] = (k == q)
    c['mcur'] = (k <= q)
    c['mprev'] = (k >= q)
    c['bones'] = (k // 64 == q // 64)
    c['ustrict'] = (k < q)
    c['ones'] = np.ones((128, 128), bool)
    return np.concatenate([c[n].astype(np.float32) for n in
                           ['ident', 'mcur', 'mprev', 'bones', 'ustrict', 'ones']], axis=1)


CI = {n: i * 128 for i, n in enumerate(['ident', 'mcur', 'mprev', 'bones', 'ustrict', 'ones'])}
NCONST = 6 * 128


def build(S, L, dbg=False, CAP=768, phases="ABCDEFG"):
    nc = bass.Bass("TRN2", target_bir_lowering=False)
    sc = Sched(nc)
    NT = S // 128
    NG = S // 512
    NCH = S // 64
    skind = "ExternalOutput" if dbg else "Internal"

    def din(name, shape, dt=F32):
        return nc.dram_tensor(name, list(shape), dt, kind="ExternalInput").ap()

    def dsc(name, shape, dt):
        return nc.dram_tensor(name, list(shape), dt, kind=skind).ap()

    x_in = din("x", [S, D])
    consts_d = din("consts", [128, NCONST])
    g_mix_d = din("g_mix_fm", [L, 128, 8])
    w_in_d = din("w_in", [L, D, DIN])
    b_if_d = din("b_if", [L, 8])
    conv_d = din("conv_fm", [L, 128, 8 * 4])
    gqk_d = din("gqk2", [L, 128, 2])
    w_out_d = din("w_out", [L, D, D])
    g_ffn_d = din("g_ffn", [L, D])
    w_r_d = din("w_r", [L, D, 36])
    b_r_d = din("b_r", [L, 36])
    w1_d = din("w1", [L, NEXP, D, DEXP])
    w3_d = din("w3", [L, NEXP, D, DEXP])
    w2_d = din("w2", [L, NEXP, DEXP, D])
    ebase_d = din("ebase", [128, 32])
    out_d = nc.dram_tensor("out", [S, D], F32, kind="ExternalOutput").ap()

    qk_att_d = dsc("qk_att", [1024, S], BF16)
    qk_ml_d = dsc("qk_ml", [1024, S], BF16)
    v_att_d = dsc("v_att", [S, 512], BF16)
    v_ml_d = dsc("v_ml", [S, 512], BF16)
    og_d = dsc("og", [S, 512], BF16)
    gates_d = dsc("gates", [S, 8], F32)
    yT_d = dsc("yT", [1024, S], BF16)
    xs_d = dsc("xs", [NEXP * CAP, D], BF16)
    ys_d = dsc("ys", [NEXP * CAP, D], BF16)

    es = ExitStack()

    def sb(name, shape, dt):
        return es.enter_context(nc.sbuf_tensor(name, list(shape), dt))

    def ps(name, shape, dt=F32):
        return es.enter_context(nc.psum_tensor(name, list(shape), dt))

    cf = sb("cf", [128, NCONST], F32)
    cb = sb("cb", [128, NCONST], BF16)
    sc.dma('sp', lambda e: e.dma_start(out=cf[:], in_=consts_d[:, :]), w=['cf'])
    sc.op('dve', lambda e: e.tensor_copy(out=cb[:], in_=cf[:]), r=['cf'], w=['cb'])

    RT = sb("RT", [128, NT, 2], F32)
    RTi = sb("RTi", [128, NT, 2], I32)

    def cF(n, p=128, q=128):
        return cf[0:p, CI[n]:CI[n] + q]

    def cB(n, p=128, q=128):
        return cb[0:p, CI[n]:CI[n] + q]

    def mm(out, lhsT, rhs, start, stop, r, w, skip=False):
        sc.op('pe', lambda e: e.matmul(out, lhsT, rhs, start=start, stop=stop, skip_group_check=skip), r=r, w=w)

    def tr(out, in_, ident, r, w):
        sc.op('pe', lambda e: e.transpose(out, in_, ident), r=r, w=w)

    def act(out, in_, func, r, w, **kw):
        sc.op('act', lambda e: e.activation(out=out, in_=in_, func=func, **kw), r=r, w=w)

    def dma(q, out, in_, r, w, **kw):
        sc.dma(q, lambda e: e.dma_start(out=out, in_=in_, **kw), r=r, w=w)

    _uq = [0]

    def uq():
        _uq[0] += 1
        return ('u', _uq[0])

    def phase_A(l, xin, fuse_g=False):
        with ExitStack() as st:
            def sbt(name, shape, dt):
                return st.enter_context(nc.sbuf_tensor(f"A{l}_{name}", list(shape), dt))

            def pst(name, shape, dt=F32):
                return st.enter_context(nc.psum_tensor(f"A{l}_{name}", list(shape), dt))
            wb = sbt("wb", [128, 8, DIN], BF16)
            gm = sbt("gm", [128, 8], F32)
            cw = sbt("cw", [128, 32], F32)
            gqk = sbt("gqk", [128, 2], F32)
            bif = sbt("bif", [128, 8], F32)
            xgl = [sbt(f"xg{i}", [128, 4, D], F32) for i in range(2)]
            junk = sbt("junk", [128, D], BF16)
            ssl = [sbt(f"ss{i}", [128, 4], F32) for i in range(2)]
            nrml = [sbt(f"nrm{i}", [128, 4], F32) for i in range(2)]
            rstdl = [sbt(f"rstd{i}", [128, 4], F32) for i in range(2)]
            xnl = [sbt(f"xn{i}", [128, 4, D], BF16) for i in range(2)]
            xnTl = [sbt(f"xnT{i}", [128, 8, 512], BF16) for i in range(2)]
            sql = [sbt(f"sq{i}", [128, 512], BF16) for i in range(2)]
            nr2l = [sbt(f"nr2{i}", [128, 512], F32) for i in range(2)]
            rinvl = [sbt(f"rinv{i}", [128, 512], F32) for i in range(2)]
            NOB = 8
            ob = [sbt(f"ob{i}", [128, 512], BF16) for i in range(NOB)]
            raw = sbt("raw", [128, 8, 515], BF16)
            dg = sbt("dg", [128, 8, 4, 128], BF16)
            gt = sbt("gt", [128, 8], F32)
            if fuse_g:
                r1l = [sbt(f"r1{i}", [128, D], BF16) for i in range(4)]
                r2l = [sbt(f"r2{i}", [128, D], BF16) for i in range(4)]
            pt = [pst(f"pt{i}", [128, 512], BF16) for i in range(2)]
            pj = [pst(f"pj{i}", [128, 512]) for i in range(3)]
            pssl = [pst(f"pss{i}", [128, 512]) for i in range(2)]
            w_src = w_in_d[l].rearrange("(kc p) n -> p kc n", p=128)
            wpieces = [(0, 512), (512, 1024), (1536, 2048), (2048, 2560), (1024, 1536), (2560, 3072), (3072, DIN)]
            def load_w_in():
                for i_, (a_, b_) in enumerate(wpieces):
                    dma('pool', wb[:, :, a_:b_], w_src[:, :, a_:b_], r=[], w=[('wb', i_)])

            def wkey(c0):
                for i_, (a_, b_) in enumerate(wpieces):
                    if a_ <= c0 < b_:
                        return ('wb', i_)
            dma('sp', gm[:], g_mix_d[l], r=[], w=['gm'])
            dma('sp', cw[:], conv_d[l], r=[], w=['cw'])
            dma('sp', gqk[:], gqk_d[l], r=[], w=['gqk'])
            dma('sp', bif[:], b_if_d[l].partition_broadcast(128), r=[], w=['bif'])
            sc.op('pool', lambda e: e.memset(raw[:], 0.0), w=['raw'])
            for cm in range(8):
                for jj in range(4):
                    sc.op('dve', lambda e: e.tensor_scalar(out=dg[:, cm, jj, :], in0=cB('ident'),
                                                           scalar1=cw[:, cm * 4 + jj:cm * 4 + jj + 1], scalar2=None,
                                                           op0=ALU.mult), r=['cb', 'cw'], w=['dg'])
            nob = [0]
            npj = [0]
            ntp = [0]

            def next_pj():
                npj[0] += 1
                i = npj[0] % 3
                return pj[i], ('pj', i)

            def next_ob():
                nob[0] += 1
                i = nob[0] % NOB
                return ob[i], ('ob', i)

            def prep_dma(g):
                gb = g % 2
                t0 = g * 512
                dma('pool', xgl[gb][:], xin[t0:t0 + 512, :].rearrange("(j p) f -> p j f", p=128), r=[], w=[('xg', gb, j_) for j_ in range(4)])
                if fuse_g:
                    for j in range(4):
                        ti = g * 4 + j
                        for (rl, k_, nm) in ((r1l, 0, 'r1'), (r2l, 1, 'r2')):
                            sc.dma('pool', lambda e: e.indirect_dma_start(
                                out=rl[ti % 4][:, :], out_offset=None, in_=ys_d[:, :],
                                in_offset=bass.IndirectOffsetOnAxis(ap=RTi[:, ti, k_:k_ + 1], axis=0)),
                                r=['RTi'], w=[(nm, ti % 4)])

            def prep_cmp(g):
                gb = g % 2
                t0 = g * 512
                xg, ss, nrm, rstd, xn, xnT = xgl[gb], ssl[gb], nrml[gb], rstdl[gb], xnl[gb], xnTl[gb]
                if fuse_g:
                    for j in range(4):
                        ti = g * 4 + j
                        for (rl, k_, nm) in ((r1l, 0, 'r1'), (r2l, 1, 'r2')):
                            sc.op('dve', lambda e: e.scalar_tensor_tensor(out=xg[:, j, :], in0=rl[ti % 4][:], scalar=RT[:, ti, k_:k_ + 1],
                                                                          in1=xg[:, j, :], op0=ALU.mult, op1=ALU.add),
                                  r=[(nm, ti % 4), 'RT', ('xg', gb, j)], w=[('xg', gb, j)])
                        dma('sp', out_d[t0 + j * 128:t0 + (j + 1) * 128, :], xg[:, j, :], r=[('xg', gb, j)], w=[uq()])
                for j in range(4):
                    act(junk[:], xg[:, j, :], AF.Square, r=[('xg', gb, j)], w=['junk', ('ss', gb, j)], accum_out=ss[:, j:j + 1])
                act(nrm[:], ss[:], AF.Sqrt, r=[('ss', gb, j) for j in range(4)], w=[('nrm', gb)], scale=1.0 / D, bias=EPS)
                sc.op('dve', lambda e: e.reciprocal(out=rstd[:], in_=nrm[:]), r=[('nrm', gb)], w=[('rstd', gb)])
                for j in range(4):
                    sc.op('dve', lambda e: e.tensor_scalar(out=xn[:, j, :], in0=xg[:, j, :], scalar1=rstd[:, j:j + 1],
                                                           scalar2=None, op0=ALU.mult),
                          r=[('xg', gb, j), ('rstd', gb)], w=[('xn', gb, j)])

            def prep_tr(g):
                gb = g % 2
                xn, xnT = xnl[gb], xnTl[gb]
                for c in range(8):
                    ntp[0] += 1
                    pi = ntp[0] % 2
                    p_ = pt[pi]
                    for j in range(4):
                        tr(p_[:, j * 128:(j + 1) * 128], xn[:, j, c * 128:(c + 1) * 128], cB('ident'),
                           r=[('xn', gb, j), 'cb'], w=[('pt', pi)])
                    sc.op('dve', lambda e: e.tensor_scalar(out=xnT[:, c, :], in0=p_[:, :], scalar1=gm[:, c:c + 1],
                                                           scalar2=None, op0=ALU.mult),
                          r=[('pt', pi), 'gm'], w=[('xnT', gb, c)])

            prep_dma(0)
            load_w_in()
            prep_cmp(0)
            prep_tr(0)
            for g in range(NG):
                t0 = g * 512
                gb = g % 2
                xnT = xnTl[gb]
                if g + 1 < NG:
                    prep_dma(g + 1)

                def proj_fm(c0):
                    p_, pr = next_pj()
                    for kc in range(8):
                        mm(p_[:, :], wb[:, kc, c0:c0 + 128], xnT[:, kc, :], kc == 0, kc == 7,
                           r=[wkey(c0), ('xnT', gb, kc)], w=[pr])
                    return p_, pr

                def qk_tail(cc, p_, pr):
                    isq = cc < 4
                    i2 = cc % 2
                    sq, nr2, rinv, pss = sql[i2], nr2l[i2], rinvl[i2], pssl[i2]
                    mm(pss[:, :], cB('bones'), sq[:], True, True, r=['cb', ('sq', i2)], w=[('pss', i2)])
                    if isq:
                        act(nr2[:], pss[:, :], AF.Ln, r=[('pss', i2)], w=[('nr2', i2)], scale=1.0, bias=64.0 * EPS)
                    else:
                        act(nr2[:], pss[:, :], AF.Ln, r=[('pss', i2)], w=[('nr2', i2)], scale=1.0 / 64.0, bias=EPS)
                    act(rinv[:], nr2[:], AF.Exp, r=[('nr2', i2)], w=[('rinv', i2)], scale=-0.5)
                    o_, orr = next_ob()
                    gcol = gqk[:, 0:1] if isq else gqk[:, 1:2]
                    sc.op('dve', lambda e: e.scalar_tensor_tensor(out=o_[:], in0=p_[:, :], scalar=gcol, in1=rinv[:],
                                                                  op0=ALU.mult, op1=ALU.mult),
                          r=[pr, ('rinv', i2), 'gqk'], w=[orr])
                    dma('sp', qk_att_d[cc * 128:(cc + 1) * 128, t0:t0 + 512], o_[:], r=[orr], w=[uq()])

                prev = None
                for cc in range(8):
                    p_, pr = proj_fm(cc * 128)
                    act(sql[cc % 2][:], p_[:, :], AF.Square, r=[pr], w=[('sq', cc % 2)])
                    if prev is not None:
                        qk_tail(*prev)
                    prev = (cc, p_, pr)
                for cm in range(8):
                    p_, pr = proj_fm(1536 + cm * 128)
                    if cm == 0:
                        qk_tail(*prev)
                    RW = ('raw', cm)
                    act(raw[:, cm, 3:515], p_[:, :], AF.Copy, r=[pr, 'raw'], w=[RW])
                    pc_, pcr = next_pj()
                    for jj in range(4):
                        mm(pc_[:, :], dg[:, cm, jj, :], raw[:, cm, jj:jj + 512], jj == 0, jj == 3, r=['dg', RW], w=[pcr])
                    o_, orr = next_ob()
                    act(o_[:], pc_[:, :], AF.Silu, r=[pcr], w=[orr])
                    dma('sp', qk_ml_d[cm * 128:(cm + 1) * 128, t0:t0 + 512], o_[:], r=[orr], w=[uq()])
                    sc.op('pool', lambda e: e.tensor_copy(out=raw[:, cm, 0:3], in_=raw[:, cm, 512:515]),
                          r=[RW], w=[RW])
                if g + 1 < NG:
                    prep_cmp(g + 1)
                for j in range(4):
                    tk = t0 + j * 128
                    if j == 2 and g + 1 < NG:
                        prep_tr(g + 1)
                    for (c0, n, kind) in [(1024, 512, 'va'), (2560, 512, 'vm'), (3072, 512, 'og'), (3584, 8, 'if')]:
                        p_, pr = next_pj()
                        for kc in range(8):
                            mm(p_[:, 0:n], xnT[:, kc, j * 128:(j + 1) * 128], wb[:, kc, c0:c0 + n], kc == 0, kc == 7,
                               r=[wkey(c0), ('xnT', gb, kc)], w=[pr])
                        if kind == 'if':
                            sc.op('dve', lambda e: e.tensor_tensor(out=gt[:], in0=p_[:, 0:8], in1=bif[:], op=ALU.add),
                                  r=[pr, 'bif'], w=['gt'])
                            dma('sp', gates_d[tk:tk + 128, :], gt[:], r=['gt'], w=[uq()])
                        else:
                            o_, orr = next_ob()
                            if kind == 'vm':
                                sc.op('dve', lambda e: e.tensor_copy(out=o_[:], in_=p_[:, :]), r=[pr], w=[orr])
                            else:
                                act(o_[:], p_[:, :], AF.Sigmoid if kind == 'og' else AF.Copy, r=[pr], w=[orr])
                            dst = {'va': v_att_d, 'vm': v_ml_d, 'og': og_d}[kind]
                            dma('sp', dst[tk:tk + 128, :], o_[:], r=[orr], w=[uq()])

    def phase_B(l):
        NSB = S // 2048
        LAG = 3
        NBUF = LAG + 1
        with ExitStack() as st:
            def sbt(name, shape, dt):
                return st.enter_context(nc.sbuf_tensor(f"B{l}_{name}", list(shape), dt))

            def pst(name, shape, dt=F32):
                return st.enter_context(nc.psum_tensor(f"B{l}_{name}", list(shape), dt))
            qTl = [sbt(f"qT{i}", [64, S], BF16) for i in range(2)]
            kTl = [sbt(f"kT{i}", [64, S], BF16) for i in range(2)]
            vdl = [{d: sbt(f"vd{d}_{i}", [128, S // 128, 128], BF16) for d in (1, 4, 16)} for i in range(2)]
            M4 = sbt("M4", [128, 640], BF16)
            M4c = sbt("M4c", [128, 512], BF16)
            ptl = [sbt(f"pt{i}", [128, 512], BF16) for i in range(NBUF)]
            rden = [sbt(f"rden{i}", [64, 512], F32) for i in range(2)]
            yo = [sbt(f"yo{i}", [64, 512], BF16) for i in range(2)]
            acc = pst("acc", [128, 4, 512])
            stl = [pst(f"st{i}", [128, 512]) for i in range(NBUF)]
            for i in range(5):
                sc.op('dve', lambda e: e.tensor_copy(out=M4[:, i * 128:(i + 1) * 128],
                                                     in_=cB('mprev' if i % 2 == 0 else 'mcur')), r=['cb'], w=['M4'])
            for i in range(4):
                sc.op('dve', lambda e: e.tensor_copy(out=M4c[:, i * 128:(i + 1) * 128], in_=cB('mcur')),
                      r=['cb'], w=['M4c'])
            for i in range(2):
                for d in (1, 4, 16):
                    sc.op('pool', lambda e: e.memset(vdl[i][d][:, :, 64:128], 1.0), w=[('vd', i, d, 'ones')])

            def tok(base, d_):
                return slice(base, base + d_ * 127 + 1, d_)

            def load_head(h):
                hb = h % 2
                qs = ['pool', 'sp'] if h == 0 else ['pool', 'pool']
                dma(qs[0], qTl[hb][:, :], qk_att_d[h * 64:(h + 1) * 64, :], r=[], w=[('qT', hb)])
                dma(qs[1], kTl[hb][:, :], qk_att_d[512 + h * 64:512 + (h + 1) * 64, :], r=[], w=[('kT', hb)])
                vsrc = v_att_d[:, h * 64:(h + 1) * 64]
                k_ = 0
                for d in (16, 4):
                    for r_ in range(d):
                        s_ap = vsrc[r_:S:d, :].rearrange("(n k) c -> k n c", k=128)
                        o_ap = vdl[hb][d][:, r_:S // 128:d, 0:64]
                        dma(qs[k_ % 2], o_ap, s_ap, r=[], w=[('vd', hb, d, r_)])
                        k_ += 1
                for sbi in range(NSB):
                    dma(qs[k_ % 2], vdl[hb][1][:, sbi * 16:(sbi + 1) * 16, 0:64],
                        vsrc[sbi * 2048:(sbi + 1) * 2048, :].rearrange("(n k) c -> k n c", k=128), r=[], w=[('vd', hb, 1, sbi)])
                    k_ += 1

            def head_items():
                items = []
                for sbi in range(NSB):
                    T0 = sbi * 2048

                    def add(pairs):
                        for g0 in range(0, len(pairs), 4):
                            items.append(('g', pairs[g0:g0 + 4], sbi))
                    pairs = []
                    for r_ in range(16):
                        outs = [(seg, slice(r_, r_ + 16 * 31 + 1, 16), slice(32 * seg, 32 * seg + 32)) for seg in range(4)]
                        if sbi > 0:
                            pairs.append((T0 + r_, T0 - 2048 + r_, 16, (sbi - 1) * 16 + r_, 'p', outs))
                        pairs.append((T0 + r_, T0 + r_, 16, sbi * 16 + r_, 'c', outs))
                    add(pairs)
                    for seg in range(4):
                        pairs = []
                        n4 = sbi * 4 + seg
                        for r_ in range(4):
                            outs = [(seg, slice(r_, r_ + 4 * 127 + 1, 4), slice(0, 128))]
                            qb = T0 + seg * 512 + r_
                            if n4 > 0:
                                pairs.append((qb, qb - 512, 4, (n4 - 1) * 4 + r_, 'p', outs))
                            pairs.append((qb, qb, 4, n4 * 4 + r_, 'c', outs))
                        for b in range(4):
                            nb = sbi * 16 + seg * 4 + b
                            outs = [(seg, slice(b * 128, (b + 1) * 128), slice(0, 128))]
                            if nb > 0:
                                pairs.append((nb * 128, (nb - 1) * 128, 1, nb - 1, 'p', outs))
                            pairs.append((nb * 128, nb * 128, 1, nb, 'c', outs))
                        add(pairs)
                        items.append(('n', seg, sbi))
                return items

            ngrp = [0]
            nyo = [0]
            touched = {}

            def stage1(h, grp):
                hb = h % 2
                qT, kT = qTl[hb], kTl[hb]
                ngrp[0] += 1
                bi = ngrp[0] % NBUF
                st_, pt_ = stl[bi], ptl[bi]
                n = len(grp)
                for i, (qb, kb, d_, vb, mt, outs) in enumerate(grp):
                    mm(st_[:, i * 128:(i + 1) * 128], kT[0:64, tok(kb, d_)], qT[0:64, tok(qb, d_)],
                       True, True, r=[('qT', hb), ('kT', hb)], w=[('st', bi)], skip=True)
                act(pt_[:, 0:n * 128], st_[:, 0:n * 128], AF.Exp, r=[('st', bi)], w=[('pt', bi)])
                types = [p[4] for p in grp]
                if all(t == 'c' for t in types):
                    msl = [(0, n, M4c[:, 0:n * 128], 'M4c')]
                elif all(types[i] != types[i + 1] for i in range(n - 1)):
                    o0 = 0 if types[0] == 'p' else 1
                    msl = [(0, n, M4[:, o0 * 128:(o0 + n) * 128], 'M4')]
                else:
                    msl = [(i, 1, M4c[:, 0:128] if types[i] == 'c' else M4[:, 0:128],
                            'M4c' if types[i] == 'c' else 'M4') for i in range(n)]
                for (i0, cnt, map_, mres) in msl:
                    sc.op('dve', lambda e: e.tensor_tensor(out=pt_[:, i0 * 128:(i0 + cnt) * 128],
                                                           in0=pt_[:, i0 * 128:(i0 + cnt) * 128],
                                                           in1=map_, op=ALU.mult),
                          r=[mres, ('pt', bi)], w=[('pt', bi)])
                return bi

            def stage2(h, grp, sbi, bi):
                hb = h % 2
                pt_ = ptl[bi]
                for i, (qb, kb, d_, vb, mt, outs) in enumerate(grp):
                    for (seg, cols, qsub) in outs:
                        key = (h, sbi, seg)
                        first = key not in touched
                        touched[key] = True
                        mm(acc[:, seg, cols], vdl[hb][d_][:, vb, :], pt_[:, i * 128 + qsub.start:i * 128 + qsub.stop],
                           first, True, r=[('vd', hb, d_, 'ones'), ('vd', hb, d_, (vb % d_) if d_ > 1 else (vb // 16)), ('pt', bi)], w=[('acc', seg)], skip=True)

            def norm(h, seg, sbi):
                T0 = sbi * 2048
                nyo[0] += 1
                yi = nyo[0] % 2
                act(rden[yi][:, :], acc[64:128, seg, :], AF.Ln, r=[('acc', seg)], w=[('rden', yi)])
                act(rden[yi][:, :], rden[yi][:, :], AF.Exp, r=[('rden', yi)], w=[('rden', yi)], scale=-1.0)
                sc.op('dve', lambda e: e.tensor_tensor(out=yo[yi][:, :], in0=acc[0:64, seg, :], in1=rden[yi][:, :],
                                                       op=ALU.mult),
                      r=[('acc', seg), ('rden', yi)], w=[('yo', yi)])
                dma('sp', yT_d[h * 64:(h + 1) * 64, T0 + seg * 512:T0 + (seg + 1) * 512], yo[yi][:, :],
                    r=[('yo', yi)], w=[uq()])

            allitems = []
            for h in range(8):
                allitems.append(('load', h))
                for it in head_items():
                    allitems.append((it[0], h) + it[1:])
            order = []
            back = [0]
            npend = [0]

            def drain(k):
                while npend[0] > k:
                    it = order[back[0]]
                    if it[0] == 'g':
                        stage2(it[1], it[2], it[3], it[4])
                        npend[0] -= 1
                    else:
                        norm(it[1], it[2], it[3])
                    back[0] += 1
                while back[0] < len(order) and order[back[0]][0] == 'n':
                    it = order[back[0]]
                    norm(it[1], it[2], it[3])
                    back[0] += 1

            load_head(0)
            cnt_h = {}
            for it in allitems:
                if it[0] == 'load':
                    continue
                if it[0] == 'g':
                    _, h, grp, sbi = it
                    bi = stage1(h, grp)
                    order.append(('g', h, grp, sbi, bi))
                    npend[0] += 1
                    cnt_h[h] = cnt_h.get(h, 0) + 1
                    if cnt_h[h] == LAG + 2 and h + 1 < 8:
                        load_head(h + 1)
                else:
                    order.append(it)
                drain(LAG)
            drain(0)

    def phase_C(l):
        CK = 128
        SEG = 2048
        NSEG = S // SEG
        CPS = SEG // CK
        C0 = -0.5 * math.log(128.0)
        with ExitStack() as st:
            def sbt(name, shape, dt):
                return st.enter_context(nc.sbuf_tensor(f"C{l}_{name}", list(shape), dt))

            def pst(name, shape, dt=F32):
                return st.enter_context(nc.psum_tensor(f"C{l}_{name}", list(shape), dt))
            qcl = [sbt(f"qc{i}", [128, 4, SEG], BF16) for i in range(2)]
            kcl = [sbt(f"kc{i}", [128, 4, SEG], BF16) for i in range(2)]
            vaugl = [sbt(f"vaug{i}", [128, CPS, 4, 132], BF16) for i in range(2)]
            ogl = [sbt(f"og{i}", [128, CPS, 512], BF16) for i in range(2)]
            gtsl = [sbt(f"gts{i}", [128, CPS, 8], F32) for i in range(2)]
            yTs = sbt("yTs", [128, 4, SEG], BF16)
            Sf = sbt("Sf", [128, 4, 132], F32)
            Sb = sbt("Sb", [128, 4, 132], BF16)
            t1 = sbt("t1", [128, CPS, 4], F32)
            sp_ = sbt("sp", [128, CPS, 4], F32)
            t3 = sbt("t3", [128, CPS, 4], F32)
            esl = [sbt(f"es{i}", [128, CPS, 4], F32) for i in range(2)]
            ebl_ = [sbt(f"eb{i}", [128, CPS, 4], F32) for i in range(2)]
            ebll = [sbt(f"ebl{i}", [128, CPS, 4], F32) for i in range(2)]
            ATm = [sbt(f"ATm{i}", [128, 4, 128], BF16) for i in range(2)]
            kp = [sbt(f"kp{i}", [128, 4, 128], BF16) for i in range(2)]
            ychl = [sbt(f"ych{i}", [128, 4, 128], BF16) for i in range(2)]
            ytmpl = [sbt(f"ytmp{i}", [128, 4, 128], F32) for i in range(2)]
            d1 = sbt("d1", [128, 4], F32)
            d2 = sbt("d2", [128, 4], F32)
            d3 = sbt("d3", [128, 4], F32)
            scl = sbt("scl", [128, 4], F32)
            pa = [pst(f"pa{i}", [128, 4, 128]) for i in range(2)]
            pkb = [pst(f"pkb{i}", [128, 8, 128], BF16) for i in range(2)]
            pr = pst("pr", [128, 4, 256])
            pu = pst("pu", [128, 4, 256])
            for i in range(2):
                sc.op('pool', lambda e: e.memset(vaugl[i][:], 1.0), w=[('vaug', i)])
            sc.op('pool', lambda e: e.memset(Sf[:], 0.0), w=['Sf'])
            sc.op('pool', lambda e: e.memset(Sb[:], 0.0), w=['Sb'])

            def dve(fn, r, w):
                sc.op('dve', fn, r=r, w=w)

            def load_seg(sg):
                b = sg % 2
                T0 = sg * SEG
                dma('sp', qcl[b][:], qk_ml_d[0:512, T0:T0 + SEG].rearrange("(h p) t -> p h t", p=128), r=[], w=[('qc', b)])
                dma('sp', kcl[b][:], qk_ml_d[512:1024, T0:T0 + SEG].rearrange("(h p) t -> p h t", p=128), r=[], w=[('kc', b)])
                for h in range(4):
                    dma('pool', vaugl[b][:, :, h, 0:128],
                        v_ml_d[T0:T0 + SEG, h * 128:(h + 1) * 128].rearrange("(c p) f -> p c f", p=128),
                        r=[], w=[('vaug', b)])
                dma('pool', ogl[b][:], og_d[T0:T0 + SEG, :].rearrange("(c p) f -> p c f", p=128), r=[], w=[('og', b)])
                dma('pool', gtsl[b][:], gates_d[T0:T0 + SEG, :].rearrange("(c p) f -> p c f", p=128), r=[], w=[('gts', b)])

            def prepass(sg):
                b = sg % 2
                gts, es_, eb, ebl = gtsl[b], esl[b], ebl_[b], ebll[b]
                act(t1[:], gts[:, :, 4:8], AF.Exp, r=[('gts', b)], w=['t1'], scale=-1.0)
                act(sp_[:], t1[:], AF.Ln, r=['t1'], w=['sp'], bias=1.0)
                spf = sp_[:].rearrange("p c h -> p (c h)")
                NW = CPS * 4
                mm(pu[:, 0, 0:NW], cF('mcur'), spf, True, True, r=['cf', 'sp'], w=['pu'])
                mm(pu[:, 1, 0:NW], cF('ones'), spf, True, True, r=['cf', 'sp'], w=['pu'])
                nb = pu[:, 0, 0:NW].rearrange("p (c h) -> p c h", h=4)
                dve(lambda e: e.tensor_tensor(out=t3[:], in0=nb, in1=gts[:, :, 0:4], op=ALU.add), r=['pu', ('gts', b)], w=['t3'])
                act(es_[:], t3[:], AF.Exp, r=['t3'], w=[('es', b)], bias=C0)
                act(eb[:], nb, AF.Exp, r=['pu'], w=[('eb', b)], scale=-1.0)
                act(ebl[:], pu[:, 1, 0:NW].rearrange("p (c h) -> p c h", h=4), AF.Exp, r=['pu'], w=[('ebl', b)], scale=-1.0)

            NCK = S // CK

            def stage1(ci):
                sg, c = divmod(ci, CPS)
                b = sg % 2
                pb = ci % 2
                lo = c * CK
                qc, kc, es_ = qcl[b], kcl[b], esl[b]
                for h in range(4):
                    mm(pa[pb][:, h, :], kc[:, h, lo:lo + CK], qc[:, h, lo:lo + CK], True, True,
                       r=[('kc', b), ('qc', b)], w=[('pa', pb)])
                for h in range(4):
                    tr(pkb[pb][:, h, :], kc[:, h, lo:lo + CK], cB('ident'), r=[('kc', b), 'cb'], w=[('pkb', pb)])
                esb = es_[:, c, :, None].to_broadcast([128, 4, 128])
                dve(lambda e: e.tensor_tensor(out=ATm[pb][:], in0=pa[pb][:], in1=esb, op=ALU.mult),
                    r=[('pa', pb), ('es', b)], w=[('ATm', pb)])
                sc.op('pool', lambda e: e.tensor_tensor(out=ATm[pb][:], in0=ATm[pb][:],
                                                        in1=cB('mcur')[:, None, :].to_broadcast([128, 4, 128]), op=ALU.mult),
                      r=[('ATm', pb), 'cb'], w=[('ATm', pb)])
                for h in range(4):
                    act(kp[pb][:, h, :], pkb[pb][:, h, :], AF.Copy, r=[('pkb', pb), ('es', b)], w=[('kp', pb)],
                        scale=es_[:, c, h:h + 1])

            def stage2(ci):
                sg, c = divmod(ci, CPS)
                b = sg % 2
                pb = ci % 2
                lo = c * CK
                qc, vaug, og, eb, ebl = qcl[b], vaugl[b], ogl[b], ebl_[b], ebll[b]
                for h in range(4):
                    mm(pr[:, h, 0:129], ATm[pb][:, h, :], vaug[:, c, h, 0:129], True, False,
                       r=[('ATm', pb), ('vaug', b)], w=['pr'])
                    mm(pr[:, h, 0:129], qc[:, h, lo:lo + CK], Sb[:, h, 0:129], False, True, r=[('qc', b), 'Sb'], w=['pr'])
                for h in range(4):
                    mm(pu[:, h, 0:129], kp[pb][:, h, :], vaug[:, c, h, 0:129], True, True, r=[('kp', pb), ('vaug', b)], w=['pu'])
                dve(lambda e: e.tensor_tensor(out=Sf[:, :, 0:129], in0=pu[:, :, 0:129], in1=Sf[:, :, 0:129], op=ALU.add),
                    r=['pu', 'Sf'], w=['Sf'])
                dve(lambda e: e.tensor_tensor(out=Sf[:, :, 0:129], in0=Sf[:, :, 0:129],
                                              in1=ebl[:, c, :, None].to_broadcast([128, 4, 129]), op=ALU.mult),
                    r=[('ebl', b), 'Sf'], w=['Sf'])
                dve(lambda e: e.tensor_tensor(out=d1[:], in0=pr[:, :, 128], in1=eb[:, c, :], op=ALU.mult),
                    r=['pr', ('eb', b)], w=['d1'])
                dve(lambda e: e.tensor_scalar(out=d2[:], in0=d1[:], scalar1=-1.0, scalar2=1.0, op0=ALU.mult, op1=ALU.max),
                    r=['d1'], w=['d2'])
                dve(lambda e: e.tensor_tensor(out=d2[:], in0=d2[:], in1=d1[:], op=ALU.max), r=['d1', 'd2'], w=['d2'])
                dve(lambda e: e.reciprocal(out=d3[:], in_=d2[:]), r=['d2'], w=['d3'])
                dve(lambda e: e.tensor_tensor(out=scl[:], in0=d3[:], in1=eb[:, c, :], op=ALU.mult), r=['d3', ('eb', b)], w=['scl'])
                act(Sb[:, :, 0:129], Sf[:, :, 0:129], AF.Copy, r=['Sf'], w=['Sb'])
                ytmp, ych = ytmpl[pb], ychl[pb]
                dve(lambda e: e.tensor_tensor(out=ytmp[:], in0=pr[:, :, 0:128], in1=scl[:, :, None].to_broadcast([128, 4, 128]),
                                              op=ALU.mult), r=['pr', 'scl'], w=[('ytmp', pb)])
                sc.op('pool', lambda e: e.tensor_tensor(out=ych[:], in0=ytmp[:], in1=og[:, c, :].rearrange("p (h f) -> p h f", h=4),
                                                        op=ALU.mult), r=[('ytmp', pb), ('og', b)], w=[('ych', pb)])

            def stage3(ci):
                sg, c = divmod(ci, CPS)
                pb = ci % 2
                lo = c * CK
                ych = ychl[pb]
                for h in range(4):
                    tr(pkb[pb][:, 4 + h, :], ych[:, h, :], cB('ident'), r=[('ych', pb), 'cb'], w=[('pkb', pb)])
                act(yTs[:, :, lo:lo + CK], pkb[pb][:, 4:8, :], AF.Copy, r=[('pkb', pb)], w=['yTs'])
                if c == CPS - 1:
                    T0 = sg * SEG
                    dma('sp', yT_d[512:1024, T0:T0 + SEG].rearrange("(h p) t -> p h t", p=128), yTs[:], r=['yTs'], w=[uq()])

            load_seg(0)
            prepass(0)
            stage1(0)
            for ci in range(NCK):
                sg, c = divmod(ci, CPS)
                if c == 0 and sg + 1 < NSEG:
                    load_seg(sg + 1)
                if ci + 1 < NCK:
                    if (ci + 1) % CPS == 0:
                        prepass((ci + 1) // CPS)
                    stage1(ci + 1)
                stage2(ci)
                if ci > 0:
                    stage3(ci - 1)
            stage3(NCK - 1)

    BIG = 1.0e4

    def phase_D(l, xin):
        with ExitStack() as st:
            def sbt(name, shape, dt):
                return st.enter_context(nc.sbuf_tensor(f"D{l}_{name}", list(shape), dt))

            def pst(name, shape, dt=F32):
                return st.enter_context(nc.psum_tensor(f"D{l}_{name}", list(shape), dt))
            wo = sbt("wo", [128, 8, D], BF16)
            gf = sbt("gf", [128, D], F32)
            wr = sbt("wr", [128, 8, 36], F32)
            br = sbt("br", [128, 36], F32)
            ebase = sbt("ebase", [128, 32], F32)
            carry = sbt("carry", [128, 32], F32)
            ygl = [sbt(f"yg{i}", [128, 8, 512], BF16) for i in range(2)]
            xgl = [sbt(f"xg{i}", [128, 4, D], F32) for i in range(2)]
            x1l = [sbt(f"x1{i}", [128, 4, D], F32) for i in range(2)]
            junk = sbt("junk", [128, D], BF16)
            s2l = [sbt(f"s2{i}", [128, 12], F32) for i in range(2)]
            xn2f = [sbt(f"xn2f{i}", [128, D], F32) for i in range(2)]
            xn2b = sbt("xn2b", [128, 4, D], BF16)
            xT = [sbt(f"xT{i}", [128, 8, 128], F32) for i in range(2)]
            lg = sbt("lg", [128, 4, 36], F32)
            gmx = sbt("gmx", [128, 4], F32)
            goh = sbt("goh", [128, 4, 4], F32)
            gsub = sbt("gsub", [128, 4, 4], F32)
            ge = sbt("ge", [128, 4, 4], F32)
            gsum = sbt("gsum", [128, 4], F32)
            ggate = sbt("ggate", [128, 4], F32)
            pen = sbt("pen", [128, 4, 4], F32)
            em = sbt("em", [128, 4, 32], F32)
            em2 = sbt("em2", [128, 4, 32], F32)
            m1 = sbt("m1", [128, 4], F32)
            m2 = sbt("m2", [128, 4], F32)
            oh1 = sbt("oh1", [128, 4, 32], F32)
            oh2 = sbt("oh2", [128, 4, 32], F32)
            dm = sbt("dm", [128, 4], F32)
            ex = sbt("ex", [128, 4], F32)
            p1 = sbt("p1", [128, 4], F32)
            aa = sbt("aa", [128, 4, 32], F32)
            slot = sbt("slot", [128, 4, 32], F32)
            crun = sbt("crun", [128, 32], F32)
            tmp = sbt("tmp", [128, 4, 32], F32)
            dstf = sbt("dstf", [128, 4, 2], F32)
            pj = [pst(f"pj{i}", [128, 512]) for i in range(4)]
            ptf = [pst(f"ptf{i}", [128, 512]) for i in range(2)]
            plg = pst("plg", [128, 4, 64])
            ppos = pst("ppos", [128, 256])
            dma('pool', wo[:], w_out_d[l].rearrange("(kc p) n -> p kc n", p=128), r=[], w=['wo'])
            dma('sp', gf[:], g_ffn_d[l].partition_broadcast(128), r=[], w=['gf'])
            dma('sp', wr[:], w_r_d[l].rearrange("(kc p) n -> p kc n", p=128), r=[], w=['wr'])
            dma('sp', br[:], b_r_d[l].partition_broadcast(128), r=[], w=['br'])
            dma('sp', ebase[:], ebase_d[:, :], r=[], w=['ebase'])
            sc.op('pool', lambda e: e.memset(carry[:], 0.0), w=['carry'])

            def dve(fn, r, w):
                sc.op('dve', fn, r=r, w=w)

            def load(g):
                t0 = g * 512
                dma('sp', ygl[g % 2][:], yT_d[:, t0:t0 + 512].rearrange("(c p) t -> p c t", p=128), r=[], w=[('yg', g % 2)])
                dma('sp', xgl[g % 2][:], xin[t0:t0 + 512, :].rearrange("(j p) f -> p j f", p=128), r=[], w=[('xg', g % 2)])
            load(0)
            npj = [0]

            def outproj(g):
                t0 = g * 512
                gb = g % 2
                yg, xg, x1, s2 = ygl[gb], xgl[gb], x1l[gb], s2l[gb]
                if g + 1 < NG:
                    load(g + 1)
                for j in range(4):
                    tk = t0 + j * 128
                    for hf in range(2):
                        npj[0] += 1
                        pi = npj[0] % 4
                        p_ = pj[pi]
                        for kc in range(8):
                            mm(p_[:, :], yg[:, kc, j * 128:(j + 1) * 128], wo[:, kc, hf * 512:(hf + 1) * 512], kc == 0, kc == 7,
                               r=[('yg', gb), 'wo'], w=[('pj', pi)])
                        dve(lambda e: e.tensor_tensor(out=x1[:, j, hf * 512:(hf + 1) * 512], in0=p_[:, :],
                                                      in1=xg[:, j, hf * 512:(hf + 1) * 512], op=ALU.add),
                            r=[('pj', pi), ('xg', gb)], w=[('x1', gb, j)])
                    dma('sp', out_d[tk:tk + 128, :], x1[:, j, :], r=[('x1', gb, j)], w=[('out_d', g * 4 + j)])
                    act(junk[:], x1[:, j, :], AF.Square, r=[('x1', gb, j)], w=['junk', ('s2a', gb, j)], accum_out=s2[:, j:j + 1])
                act(s2[:, 4:8], s2[:, 0:4], AF.Sqrt, r=[('s2a', gb, j) for j in range(4)], w=[('s2b', gb)], scale=1.0 / D, bias=EPS)
                dve(lambda e: e.reciprocal(out=s2[:, 8:12], in_=s2[:, 4:8]), r=[('s2b', gb)], w=[('s2c', gb)])

            outproj(0)
            for g in range(NG):
                t0 = g * 512
                gb = g % 2
                x1, s2 = x1l[gb], s2l[gb]
                if g + 1 < NG:
                    outproj(g + 1)
                for j in range(4):
                    xf = xn2f[j % 2]
                    XF = ('xn2f', j % 2)
                    dve(lambda e: e.scalar_tensor_tensor(out=xf[:], in0=x1[:, j, :], scalar=s2[:, 8 + j:9 + j], in1=gf[:],
                                                         op0=ALU.mult, op1=ALU.mult), r=[('x1', gb, j), ('s2c', gb), 'gf'], w=[XF])
                    act(xn2b[:, j, :], xf[:], AF.Copy, r=[XF], w=[('xn2b', j)])
                    for c in range(8):
                        tr(ptf[c // 4][:, (c % 4) * 128:(c % 4 + 1) * 128], xf[:, c * 128:(c + 1) * 128], cF('ident'),
                           r=[XF, 'cf'], w=[('ptf', c // 4)])
                    for hf in range(2):
                        act(xT[j % 2][:, hf * 4:(hf + 1) * 4, :], ptf[hf][:, :].rearrange("p (c t) -> p c t", t=128), AF.Copy,
                            r=[('ptf', hf)], w=[('xT', j % 2)])
                    for c in range(8):
                        mm(plg[:, j, 0:36], xT[j % 2][:, c, :], wr[:, c, :], c == 0, c == 7, r=[('xT', j % 2), 'wr'], w=['plg'])
                dve(lambda e: e.tensor_tensor(out=lg[:], in0=plg[:, :, 0:36], in1=br[:, None, :].to_broadcast([128, 4, 36]),
                                              op=ALU.add), r=['plg', 'br'], w=['lg'])
                dve(lambda e: e.tensor_reduce(out=gmx[:], in_=lg[:, :, 0:4], axis=AX.X, op=ALU.max), r=['lg'], w=['gmx'])
                dve(lambda e: e.tensor_tensor(out=goh[:], in0=lg[:, :, 0:4], in1=gmx[:, :, None].to_broadcast([128, 4, 4]),
                                              op=ALU.is_equal), r=['lg', 'gmx'], w=['goh'])
                dve(lambda e: e.tensor_tensor(out=gsub[:], in0=lg[:, :, 0:4], in1=gmx[:, :, None].to_broadcast([128, 4, 4]),
                                              op=ALU.subtract), r=['lg', 'gmx'], w=['gsub'])
                act(ge[:], gsub[:], AF.Exp, r=['gsub'], w=['ge'])
                dve(lambda e: e.tensor_reduce(out=gsum[:], in_=ge[:], axis=AX.X, op=ALU.add), r=['ge'], w=['gsum'])
                dve(lambda e: e.reciprocal(out=ggate[:], in_=gsum[:]), r=['gsum'], w=['ggate'])
                dve(lambda e: e.tensor_scalar(out=pen[:], in0=goh[:], scalar1=BIG, scalar2=-BIG, op0=ALU.mult, op1=ALU.add),
                    r=['goh'], w=['pen'])
                dve(lambda e: e.tensor_tensor(out=em[:].rearrange("p j (g e) -> p j g e", e=8),
                                              in0=lg[:, :, 4:36].rearrange("p j (g e) -> p j g e", e=8),
                                              in1=pen[:, :, :, None].to_broadcast([128, 4, 4, 8]), op=ALU.add),
                    r=['lg', 'pen'], w=['em'])
                dve(lambda e: e.tensor_reduce(out=m1[:], in_=em[:], axis=AX.X, op=ALU.max), r=['em'], w=['m1'])
                dve(lambda e: e.tensor_tensor(out=oh1[:], in0=em[:], in1=m1[:, :, None].to_broadcast([128, 4, 32]),
                                              op=ALU.is_equal), r=['em', 'm1'], w=['oh1'])
                dve(lambda e: e.scalar_tensor_tensor(out=em2[:], in0=oh1[:], scalar=-BIG, in1=em[:], op0=ALU.mult, op1=ALU.add),
                    r=['oh1', 'em'], w=['em2'])
                dve(lambda e: e.tensor_reduce(out=m2[:], in_=em2[:], axis=AX.X, op=ALU.max), r=['em2'], w=['m2'])
                dve(lambda e: e.tensor_tensor(out=oh2[:], in0=em2[:], in1=m2[:, :, None].to_broadcast([128, 4, 32]),
                                              op=ALU.is_equal), r=['em2', 'm2'], w=['oh2'])
                dve(lambda e: e.tensor_tensor(out=dm[:], in0=m2[:], in1=m1[:], op=ALU.subtract), r=['m1', 'm2'], w=['dm'])
                act(ex[:], dm[:], AF.Exp, r=['dm'], w=['ex'])
                dve(lambda e: e.tensor_scalar(out=p1[:], in0=ex[:], scalar1=1.0, scalar2=None, op0=ALU.add), r=['ex'], w=['p1'])
                dve(lambda e: e.reciprocal(out=p1[:], in_=p1[:]), r=['p1'], w=['p1'])
                dve(lambda e: e.tensor_tensor(out=RT[:, g * 4:g * 4 + 4, 0], in0=p1[:], in1=ggate[:], op=ALU.mult),
                    r=['p1', 'ggate'], w=['RT'])
                dve(lambda e: e.tensor_tensor(out=RT[:, g * 4:g * 4 + 4, 1], in0=RT[:, g * 4:g * 4 + 4, 0], in1=ex[:], op=ALU.mult),
                    r=['ex', 'RT'], w=['RT'])
                dve(lambda e: e.tensor_tensor(out=aa[:], in0=oh1[:], in1=oh2[:], op=ALU.add), r=['oh1', 'oh2'], w=['aa'])
                aaf = aa[:].rearrange("p j e -> p (j e)")
                mm(ppos[:, 0:128], cF('ustrict'), aaf, True, True, r=['cf', 'aa'], w=['ppos'])
                mm(ppos[:, 128:256], cF('ones'), aaf, True, True, r=['cf', 'aa'], w=['ppos'])
                for j in range(4):
                    dve(lambda e: e.tensor_tensor(out=slot[:, j, :], in0=ppos[:, j * 32:(j + 1) * 32], in1=carry[:], op=ALU.add),
                        r=['ppos', 'carry'], w=[('slot', j)])
                    dve(lambda e: e.tensor_tensor(out=carry[:], in0=ppos[:, 128 + j * 32:128 + (j + 1) * 32], in1=carry[:],
                                                  op=ALU.add), r=['ppos', 'carry'], w=['carry'])
                SL = [('slot', j) for j in range(4)]
                dve(lambda e: e.tensor_scalar(out=slot[:], in0=slot[:], scalar1=float(CAP - 1), scalar2=None, op0=ALU.min),
                    r=SL, w=SL)
                dve(lambda e: e.tensor_tensor(out=slot[:], in0=slot[:], in1=ebase[:, None, :].to_broadcast([128, 4, 32]), op=ALU.add),
                    r=SL + ['ebase'], w=SL)
                for k_, oh in enumerate((oh1, oh2)):
                    dve(lambda e: e.tensor_tensor(out=tmp[:], in0=slot[:], in1=oh[:], op=ALU.mult),
                        r=SL + ['oh1', 'oh2'], w=['tmp'])
                    dve(lambda e: e.tensor_reduce(out=dstf[:, :, k_], in_=tmp[:], axis=AX.X, op=ALU.add), r=['tmp'], w=['dstf'])
                dve(lambda e: e.tensor_copy(out=RTi[:, g * 4:g * 4 + 4, :], in_=dstf[:]), r=['dstf'], w=['RTi'])
                for j in range(4):
                    for k_ in range(2):
                        sc.dma('pool', lambda e: e.indirect_dma_start(
                            out=xs_d[:, :], out_offset=bass.IndirectOffsetOnAxis(ap=RTi[:, g * 4 + j, k_:k_ + 1], axis=0),
                            in_=xn2b[:, j, :], in_offset=None), r=['RTi', ('xn2b', j)], w=[uq()])

    def phase_F(l):
        GS = 384
        NGR = CAP // GS
        NB = GS // 128
        with ExitStack() as st:
            def sbt(name, shape, dt):
                return st.enter_context(nc.sbuf_tensor(f"F{l}_{name}", list(shape), dt))

            def pst(name, shape, dt=F32):
                return st.enter_context(nc.psum_tensor(f"F{l}_{name}", list(shape), dt))
            w1b = [sbt(f"w1b{i}", [128, 8, DEXP], BF16) for i in range(2)]
            w3b = [sbt(f"w3b{i}", [128, 8, DEXP], BF16) for i in range(2)]
            w2b = [sbt(f"w2b{i}", [128, 4, D], BF16) for i in range(2)]
            xsg = [sbt(f"xsg{i}", [128, NB, D], BF16) for i in range(2)]
            xT = [sbt(f"xT{i}", [128, 8, GS], BF16) for i in range(2)]
            s1 = [sbt(f"s1{i}", [128, GS], F32) for i in range(2)]
            hT = [sbt(f"hT{i}", [128, 4, GS], BF16) for i in range(2)]
            yb = [sbt(f"yb{i}", [128, D], BF16) for i in range(4)]
            ptx = [pst(f"ptx{i}", [128, 8, 128], BF16) for i in range(2)]
            ph1 = [pst(f"ph1{i}", [128, 512]) for i in range(2)]
            ph3 = [pst(f"ph3{i}", [128, 512]) for i in range(2)]
            pyy = [pst(f"pyy{i}", [128, 512]) for i in range(2)]
            nyb = [0]
            ntx = [0]
            nh = [0]
            gidx = 0
            groups = [(ex, gi) for ex in range(NEXP) for gi in range(NGR)]

            def load_w(ex):
                wi = ex % 2
                dma('pool', w1b[wi][:], w1_d[l, ex].rearrange("(kc p) n -> p kc n", p=128), r=[], w=[('w1b', wi)])
                dma('pool', w3b[wi][:], w3_d[l, ex].rearrange("(kc p) n -> p kc n", p=128), r=[], w=[('w3b', wi)])
                dma('pool', w2b[wi][:], w2_d[l, ex].rearrange("(kc p) n -> p kc n", p=128), r=[], w=[('w2b', wi)])

            def load_x(k):
                ex, gi = groups[k]
                r0 = ex * CAP + gi * GS
                dma('sp', xsg[k % 2][:], xs_d[r0:r0 + GS, :].rearrange("(b p) f -> p b f", p=128), r=[], w=[('xsg', k % 2)])

            load_w(0)
            load_x(0)
            for k, (ex, gi) in enumerate(groups):
                wi = ex % 2
                gb = k % 2
                r0 = ex * CAP + gi * GS
                if gi == 0 and ex + 1 < NEXP:
                    load_w(ex + 1)
                if k + 1 < len(groups):
                    load_x(k + 1)
                for b in range(NB):
                    ntx[0] += 1
                    ti = ntx[0] % 2
                    for c in range(8):
                        tr(ptx[ti][:, c, :], xsg[gb][:, b, c * 128:(c + 1) * 128], cB('ident'), r=[('xsg', gb), 'cb'], w=[('ptx', ti)])
                    if b % 2 == 0:
                        act(xT[gb][:, :, b * 128:(b + 1) * 128], ptx[ti][:, :, :], AF.Copy, r=[('ptx', ti)], w=[('xT', gb)])
                    else:
                        sc.op('dve', lambda e: e.tensor_copy(out=xT[gb][:, :, b * 128:(b + 1) * 128], in_=ptx[ti][:, :, :]),
                              r=[('ptx', ti)], w=[('xT', gb)])
                for m_ in range(4):
                    nh[0] += 1
                    hi = nh[0] % 2
                    for kc in range(8):
                        mm(ph1[hi][:, 0:GS], w1b[wi][:, kc, m_ * 128:(m_ + 1) * 128], xT[gb][:, kc, :], kc == 0, kc == 7,
                           r=[('w1b', wi), ('xT', gb)], w=[('ph1', hi)])
                    for kc in range(8):
                        mm(ph3[hi][:, 0:GS], w3b[wi][:, kc, m_ * 128:(m_ + 1) * 128], xT[gb][:, kc, :], kc == 0, kc == 7,
                           r=[('w3b', wi), ('xT', gb)], w=[('ph3', hi)])
                    act(s1[hi][:], ph1[hi][:, 0:GS], AF.Silu, r=[('ph1', hi)], w=[('s1', hi)])
                    sc.op('dve', lambda e: e.tensor_tensor(out=hT[gb][:, m_, :], in0=ph3[hi][:, 0:GS], in1=s1[hi][:], op=ALU.mult),
                          r=[('ph3', hi), ('s1', hi)], w=[('hT', gb, m_)])
                for b in range(NB):
                    nyb[0] += 1
                    yi = nyb[0] % 4
                    for hf in range(2):
                        for m_ in range(4):
                            mm(pyy[hf][:, :], hT[gb][:, m_, b * 128:(b + 1) * 128], w2b[wi][:, m_, hf * 512:(hf + 1) * 512],
                               m_ == 0, m_ == 3, r=[('hT', gb, m_), ('w2b', wi)], w=[('pyy', hf)])
                        if hf == 0:
                            act(yb[yi][:, 0:512], pyy[hf][:, :], AF.Copy, r=[('pyy', hf)], w=[('yb', yi)])
                        else:
                            sc.op('dve', lambda e: e.tensor_copy(out=yb[yi][:, 512:1024], in_=pyy[hf][:, :]),
                                  r=[('pyy', hf)], w=[('yb', yi)])
                    dma('sp', ys_d[r0 + b * 128:r0 + (b + 1) * 128, :], yb[yi][:], r=[('yb', yi)], w=[uq()])

    def phase_G(l):
        with ExitStack() as st:
            def sbt(name, shape, dt):
                return st.enter_context(nc.sbuf_tensor(f"G{l}_{name}", list(shape), dt))
            r1 = [sbt(f"r1{i}", [128, D], BF16) for i in range(4)]
            r2 = [sbt(f"r2{i}", [128, D], BF16) for i in range(4)]
            xx = [sbt(f"xx{i}", [128, D], F32) for i in range(4)]
            for ti in range(NT):
                i = ti % 4
                tk = ti * 128
                sc.dma('pool', lambda e: e.indirect_dma_start(
                    out=r1[i][:, :], out_offset=None, in_=ys_d[:, :],
                    in_offset=bass.IndirectOffsetOnAxis(ap=RTi[:, ti, 0:1], axis=0)), r=['RTi'], w=[('r1', i)])
                sc.dma('pool', lambda e: e.indirect_dma_start(
                    out=r2[i][:, :], out_offset=None, in_=ys_d[:, :],
                    in_offset=bass.IndirectOffsetOnAxis(ap=RTi[:, ti, 1:2], axis=0)), r=['RTi'], w=[('r2', i)])
                dma('act', xx[i][:], out_d[tk:tk + 128, :], r=[], w=[('xx', i)])
                sc.op('dve', lambda e: e.scalar_tensor_tensor(out=xx[i][:], in0=r1[i][:], scalar=RT[:, ti, 0:1], in1=xx[i][:],
                                                              op0=ALU.mult, op1=ALU.add), r=[('r1', i), 'RT', ('xx', i)], w=[('xx', i)])
                sc.op('dve', lambda e: e.scalar_tensor_tensor(out=xx[i][:], in0=r2[i][:], scalar=RT[:, ti, 1:2], in1=xx[i][:],
                                                              op0=ALU.mult, op1=ALU.add), r=[('r2', i), 'RT', ('xx', i)], w=[('xx', i)])
                dma('sp', out_d[tk:tk + 128, :], xx[i][:], r=[('xx', i)], w=[('out_d', ti)])

    xin = x_in
    for l in range(L):
        if 'A' in phases:
            phase_A(l, xin, fuse_g=(l > 0 and 'G' in phases))
            sc.barrier()
        if 'B' in phases:
            phase_B(l)
            sc.barrier()
        if 'C' in phases:
            phase_C(l)
            sc.barrier()
        if 'D' in phases:
            phase_D(l, xin)
            sc.barrier()
        if 'F' in phases:
            phase_F(l)
            sc.barrier()
        if 'G' in phases and l == L - 1:
            phase_G(l)
            sc.barrier()
        xin = out_d

    final = ['out_d', 'qk_att_d', 'qk_ml_d', 'va_d', 'vm_d', 'og_d', 'gates_d', 'yT_d']
    sc.finish(final)
    es.close()
    return nc, sc


def host_layout(inp, L, CAP=768):
    f = lambda a: np.ascontiguousarray(np.asarray(a, dtype=np.float32))
    m = {}
    m['consts'] = host_consts()
    m['ebase'] = np.tile((np.arange(32, dtype=np.float32) * CAP)[None, :], (128, 1))
    m['g_mix_fm'] = f(inp['g_norm_mix'][:L].reshape(L, 8, 128).transpose(0, 2, 1))
    m['w_in'] = f(inp['w_in'][:L])
    m['b_if'] = f(np.concatenate([inp['b_igate'][:L], inp['b_fgate'][:L]], axis=1))
    m['conv_fm'] = f(inp['conv_w'][:L].reshape(L, 4, 8, 128).transpose(0, 3, 2, 1).reshape(L, 128, 32))
    gq = np.tile(inp['g_q'][:L], (1, 2))[:, :, None]
    gk = np.tile(inp['g_k'][:L], (1, 2))[:, :, None]
    m['gqk2'] = f(np.concatenate([gq, gk], axis=2))
    m['w_out'] = f(inp['w_out'][:L])
    m['g_ffn'] = f(inp['g_norm_ffn'][:L])
    m['w_r'] = f(np.concatenate([inp['w_group'][:L], inp['w_expert_router'][:L]], axis=2))
    m['b_r'] = f(np.concatenate([inp['b_group'][:L], inp['b_expert_router'][:L]], axis=1))
    m['w1'] = f(inp['w1'][:L])
    m['w3'] = f(inp['w3'][:L])
    m['w2'] = f(inp['w2'][:L])
    return m


SEQ = 8192
DEPTH = 4
BATCH = 4
CAPACITY = 768


def kernel(**inputs):
    inp = {k: np.asarray(v) for k, v in inputs.items()}
    B, S, _ = inp['x'].shape
    L = inp['w_in'].shape[0]
    nc, _sc = build(S, L, dbg=False, CAP=CAPACITY)
    shared = host_layout(inp, L, CAPACITY)
    in_maps = []
    for b in range(B):
        m = dict(shared)
        m['x'] = np.ascontiguousarray(inp['x'][b], dtype=np.float32)
        in_maps.append(m)
    res = run_bass_kernel_spmd(nc, in_maps, core_ids=list(range(B)))
    out = np.stack([np.asarray(r['out'], dtype=np.float32) for r in res.results], axis=0)
    return out
```

```python
import math
from contextlib import ExitStack
import numpy as np
import concourse.bass as bass
import concourse.mybir as mybir
from concourse.bass_utils import run_bass_kernel_spmd

F32 = mybir.dt.float32
BF16 = mybir.dt.bfloat16
I32 = mybir.dt.int32
U32 = mybir.dt.uint32
AF = mybir.ActivationFunctionType
ALU = mybir.AluOpType
AX = mybir.AxisListType

D = 1024
DIN = 3592
NEXP = 32
DEXP = 512
EPS = 1e-6
SEM_LIM = 30000


class Sched:
    def __init__(self, nc):
        self.nc = nc
        self.eng = {'pe': nc.tensor, 'act': nc.scalar, 'dve': nc.vector, 'pool': nc.gpsimd, 'sp': nc.sync}
        self.cnt = {e: 0 for e in self.eng}
        self.sems = {e: [] for e in self.eng}
        self.seen = {e: {} for e in self.eng}
        self.lastw = {}
        self.readers = {}
        self.dma_sems = {}
        self.dma_rr = {}
        self.nwaits = 0
        self.ninst = 0

    def _sem(self, e, n):
        k = (n - 1) // SEM_LIM
        while len(self.sems[e]) <= k:
            self.sems[e].append(self.nc.alloc_semaphore(f"s_{e}_{len(self.sems[e])}"))
        return self.sems[e][k], (n - 1) % SEM_LIM + 1

    def _wait(self, e, evs):
        need = {}
        for ev in evs:
            if ev is None:
                continue
            if ev[0] == 'e':
                _, e2, n = ev
                if e2 == 'pe' and e == 'pe':
                    continue
                key = ('e', e2)
            else:
                _, si, n = ev
                key = ('d', si)
            if n > need.get(key, 0):
                need[key] = n
        for key, n in need.items():
            if self.seen[e].get(key, 0) >= n:
                continue
            if key[0] == 'e':
                sem, val = self._sem(key[1], n)
            else:
                sem, val = self.dma_sem_handles[key[1]], n
            self.eng[e].wait_ge(sem, val)
            self.nwaits += 1
            self.seen[e][key] = n

    def _deps(self, r, w):
        evs = []
        for res in r:
            evs.append(self.lastw.get(res))
        for res in w:
            evs.append(self.lastw.get(res))
            evs.extend(self.readers.get(res, {}).values())
        return evs

    def _commit(self, ev, r, w):
        for res in w:
            self.lastw[res] = ev
            self.readers[res] = {}
        for res in r:
            if res in w:
                continue
            self.readers.setdefault(res, {})[ev[:2]] = ev

    def op(self, e, build, r=(), w=()):
        self._wait(e, self._deps(r, w))
        ins = build(self.eng[e])
        n = self.cnt[e] + 1
        self.cnt[e] = n
        sem, _ = self._sem(e, n)
        ins.then_inc(sem, 1)
        self.ninst += 1
        self._commit(('e', e, n), r, w)

    dma_sem_handles = None

    def dma(self, q, build, r=(), w=()):
        if self.dma_sem_handles is None:
            self.dma_sem_handles = []
            self.dma_val = []
        pool = self.dma_sems.setdefault(q, [])
        if len(pool) < 8:
            si = len(self.dma_sem_handles)
            self.dma_sem_handles.append(self.nc.alloc_semaphore(f"d_{q}_{len(pool)}"))
            self.dma_val.append(0)
            pool.append(si)
            self.dma_rr[q] = len(pool) - 1
        else:
            self.dma_rr[q] = (self.dma_rr[q] + 1) % len(pool)
            si = pool[self.dma_rr[q]]
        evs = self._deps(r, w)
        if self.dma_val[si] > 0:
            evs.append(('d', si, self.dma_val[si]))
        self._wait(q, evs)
        ins = build(self.eng[q])
        self.dma_val[si] += 16
        ins.then_inc(self.dma_sem_handles[si], 16)
        self.ninst += 1
        self._commit(('d', si, self.dma_val[si]), r, w)

    def dma_bulk(self, q, builders):
        if self.dma_sem_handles is None:
            self.dma_sem_handles = []
            self.dma_val = []
        si = len(self.dma_sem_handles)
        self.dma_sem_handles.append(self.nc.alloc_semaphore(f"dbulk_{si}"))
        self.dma_val.append(0)
        for b in builders:
            ins = b(self.eng[q])
            self.dma_val[si] += 16
            ins.then_inc(self.dma_sem_handles[si], 16)
            self.ninst += 1

    def barrier(self):
        evs = []
        for e in self.eng:
            if self.cnt[e] > 0:
                evs.append(('e', e, self.cnt[e]))
        for si, v in enumerate(self.dma_val or []):
            if v > 0:
                evs.append(('d', si, v))
        for e in self.eng:
            self._wait(e, evs)

    def finish(self, res_list):
        evs = [self.lastw.get(r) for r in res_list]
        for e in self.eng:
            if self.cnt[e] > 0:
                evs.append(('e', e, self.cnt[e]))
        for si, v in enumerate(self.dma_val or []):
            if v > 0:
                evs.append(('d', si, v))
        self._wait('sp', evs)
        self.eng['sp'].nop() if hasattr(self.eng['sp'], 'nop') else None


def host_consts():
    k = np.arange(128)[:, None]
    q = np.arange(128)[None, :]
    c = {}
    c['ident'] = (k == q)
    c['mcur'] = (k <= q)
    c['mprev'] = (k >= q)
    c['bones'] = (k // 64 == q // 64)
    c['ustrict'] = (k < q)
    c['ones'] = np.ones((128, 128), bool)
    return np.concatenate([c[n].astype(np.float32) for n in
                           ['ident', 'mcur', 'mprev', 'bones', 'ustrict', 'ones']], axis=1)


CI = {n: i * 128 for i, n in enumerate(['ident', 'mcur', 'mprev', 'bones', 'ustrict', 'ones'])}
NCONST = 6 * 128


def build(S, L, dbg=False, CAP=768, phases="ABCDEFG"):
    nc = bass.Bass("TRN2", target_bir_lowering=False)
    sc = Sched(nc)
    NT = S // 128
    NG = S // 512
    NCH = S // 64
    skind = "ExternalOutput" if dbg else "Internal"

    def din(name, shape, dt=F32):
        return nc.dram_tensor(name, list(shape), dt, kind="ExternalInput").ap()

    def dsc(name, shape, dt):
        return nc.dram_tensor(name, list(shape), dt, kind=skind).ap()

    x_in = din("x", [S, D])
    consts_d = din("consts", [128, NCONST])
    g_mix_d = din("g_mix_fm", [L, 128, 8])
    w_in_d = din("w_in", [L, D, DIN])
    b_if_d = din("b_if", [L, 8])
    conv_d = din("conv_fm", [L, 128, 8 * 4])
    gqk_d = din("gqk2", [L, 128, 2])
    w_out_d = din("w_out", [L, D, D])
    g_ffn_d = din("g_ffn", [L, D])
    w_r_d = din("w_r", [L, D, 36])
    b_r_d = din("b_r", [L, 36])
    w1_d = din("w1", [L, NEXP, D, DEXP])
    w3_d = din("w3", [L, NEXP, D, DEXP])
    w2_d = din("w2", [L, NEXP, DEXP, D])
    ebase_d = din("ebase", [128, 32])
    out_d = nc.dram_tensor("out", [S, D], F32, kind="ExternalOutput").ap()

    qk_att_d = dsc("qk_att", [1024, S], BF16)
    qk_ml_d = dsc("qk_ml", [1024, S], BF16)
    v_att_d = dsc("v_att", [S, 512], BF16)
    v_ml_d = dsc("v_ml", [S, 512], BF16)
    og_d = dsc("og", [S, 512], BF16)
    gates_d = dsc("gates", [S, 8], F32)
    yT_d = dsc("yT", [1024, S], BF16)
    xs_d = dsc("xs", [NEXP * CAP, D], BF16)
    ys_d = dsc("ys", [NEXP * CAP, D], BF16)

    es = ExitStack()

    def sb(name, shape, dt):
        return es.enter_context(nc.sbuf_tensor(name, list(shape), dt))

    def ps(name, shape, dt=F32):
        return es.enter_context(nc.psum_tensor(name, list(shape), dt))

    cf = sb("cf", [128, NCONST], F32)
    cb = sb("cb", [128, NCONST], BF16)
    sc.dma('sp', lambda e: e.dma_start(out=cf[:], in_=consts_d[:, :]), w=['cf'])
    sc.op('dve', lambda e: e.tensor_copy(out=cb[:], in_=cf[:]), r=['cf'], w=['cb'])

    zt = sb("zt", [128, 4 * D], BF16)
    RT = sb("RT", [128, NT, 2], F32)
    RTi = sb("RTi", [128, NT, 2], I32)

    def cF(n, p=128, q=128):
        return cf[0:p, CI[n]:CI[n] + q]

    def cB(n, p=128, q=128):
        return cb[0:p, CI[n]:CI[n] + q]

    def mm(out, lhsT, rhs, start, stop, r, w, skip=False):
        sc.op('pe', lambda e: e.matmul(out, lhsT, rhs, start=start, stop=stop, skip_group_check=skip), r=r, w=w)

    def tr(out, in_, ident, r, w):
        sc.op('pe', lambda e: e.transpose(out, in_, ident), r=r, w=w)

    def act(out, in_, func, r, w, **kw):
        sc.op('act', lambda e: e.activation(out=out, in_=in_, func=func, **kw), r=r, w=w)

    def dma(q, out, in_, r, w, **kw):
        sc.dma(q, lambda e: e.dma_start(out=out, in_=in_, **kw), r=r, w=w)

    _uq = [0]

    def uq():
        _uq[0] += 1
        return ('u', _uq[0])

    def phase_A(l, xin, fuse_g=False):
        with ExitStack() as st:
            def sbt(name, shape, dt):
                return st.enter_context(nc.sbuf_tensor(f"A{l}_{name}", list(shape), dt))

            def pst(name, shape, dt=F32):
                return st.enter_context(nc.psum_tensor(f"A{l}_{name}", list(shape), dt))
            wb = sbt("wb", [128, 8, DIN], BF16)
            gm = sbt("gm", [128, 8], F32)
            cw = sbt("cw", [128, 32], F32)
            gqk = sbt("gqk", [128, 2], F32)
            bif = sbt("bif", [128, 8], F32)
            xgl = [sbt(f"xg{i}", [128, 4, D], F32) for i in range(2)]
            junk = sbt("junk", [128, D], BF16)
            ssl = [sbt(f"ss{i}", [128, 4], F32) for i in range(2)]
            nrml = [sbt(f"nrm{i}", [128, 4], F32) for i in range(2)]
            rstdl = [sbt(f"rstd{i}", [128, 4], F32) for i in range(2)]
            xnl = [sbt(f"xn{i}", [128, 4, D], BF16) for i in range(2)]
            xnTl = [sbt(f"xnT{i}", [128, 8, 512], BF16) for i in range(2)]
            sql = [sbt(f"sq{i}", [128, 512], BF16) for i in range(2)]
            nr2l = [sbt(f"nr2{i}", [128, 512], F32) for i in range(2)]
            rinvl = [sbt(f"rinv{i}", [128, 512], F32) for i in range(2)]
            NOB = 8
            ob = [sbt(f"ob{i}", [128, 512], BF16) for i in range(NOB)]
            raw = sbt("raw", [128, 8, 515], BF16)
            dg = sbt("dg", [128, 8, 4, 128], BF16)
            gt = sbt("gt", [128, 8], F32)
            if fuse_g:
                r1l = [sbt(f"r1{i}", [128, D], BF16) for i in range(4)]
                r2l = [sbt(f"r2{i}", [128, D], BF16) for i in range(4)]
            pt = [pst(f"pt{i}", [128, 512], BF16) for i in range(2)]
            pj = [pst(f"pj{i}", [128, 512]) for i in range(3)]
            pssl = [pst(f"pss{i}", [128, 512]) for i in range(2)]
            w_src = w_in_d[l].rearrange("(kc p) n -> p kc n", p=128)
            wpieces = [(0, 512), (512, 1024), (1536, 2048), (2048, 2560), (1024, 1536), (2560, 3072), (3072, DIN)]
            for i_, (a_, b_) in enumerate(wpieces):
                dma('pool', wb[:, :, a_:b_], w_src[:, :, a_:b_], r=[], w=[('wb', i_)])

            def wkey(c0):
                for i_, (a_, b_) in enumerate(wpieces):
                    if a_ <= c0 < b_:
                        return ('wb', i_)
            dma('sp', gm[:], g_mix_d[l], r=[], w=['gm'])
            dma('sp', cw[:], conv_d[l], r=[], w=['cw'])
            dma('sp', gqk[:], gqk_d[l], r=[], w=['gqk'])
            dma('sp', bif[:], b_if_d[l].partition_broadcast(128), r=[], w=['bif'])
            sc.op('pool', lambda e: e.memset(raw[:], 0.0), w=['raw'])
            for cm in range(8):
                for jj in range(4):
                    sc.op('dve', lambda e: e.tensor_scalar(out=dg[:, cm, jj, :], in0=cB('ident'),
                                                           scalar1=cw[:, cm * 4 + jj:cm * 4 + jj + 1], scalar2=None,
                                                           op0=ALU.mult), r=['cb', 'cw'], w=['dg'])
            nob = [0]
            npj = [0]
            ntp = [0]

            def next_pj():
                npj[0] += 1
                i = npj[0] % 3
                return pj[i], ('pj', i)

            def next_ob():
                nob[0] += 1
                i = nob[0] % NOB
                return ob[i], ('ob', i)

            def prep_dma(g):
                gb = g % 2
                t0 = g * 512
                dma('pool', xgl[gb][:], xin[t0:t0 + 512, :].rearrange("(j p) f -> p j f", p=128), r=[], w=[('xg', gb, j_) for j_ in range(4)])
                if fuse_g:
                    for j in range(4):
                        ti = g * 4 + j
                        for (rl, k_, nm) in ((r1l, 0, 'r1'), (r2l, 1, 'r2')):
                            sc.dma('pool', lambda e: e.indirect_dma_start(
                                out=rl[ti % 4][:, :], out_offset=None, in_=ys_d[:, :],
                                in_offset=bass.IndirectOffsetOnAxis(ap=RTi[:, ti, k_:k_ + 1], axis=0)),
                                r=['RTi'], w=[(nm, ti % 4)])

            def prep_cmp(g):
                gb = g % 2
                t0 = g * 512
                xg, ss, nrm, rstd, xn, xnT = xgl[gb], ssl[gb], nrml[gb], rstdl[gb], xnl[gb], xnTl[gb]
                if fuse_g:
                    for j in range(4):
                        ti = g * 4 + j
                        for (rl, k_, nm) in ((r1l, 0, 'r1'), (r2l, 1, 'r2')):
                            sc.op('dve', lambda e: e.scalar_tensor_tensor(out=xg[:, j, :], in0=rl[ti % 4][:], scalar=RT[:, ti, k_:k_ + 1],
                                                                          in1=xg[:, j, :], op0=ALU.mult, op1=ALU.add),
                                  r=[(nm, ti % 4), 'RT', ('xg', gb, j)], w=[('xg', gb, j)])
                        dma('sp', out_d[t0 + j * 128:t0 + (j + 1) * 128, :], xg[:, j, :], r=[('xg', gb, j)], w=[uq()])
                for j in range(4):
                    act(junk[:], xg[:, j, :], AF.Square, r=[('xg', gb, j)], w=['junk', ('ss', gb, j)], accum_out=ss[:, j:j + 1])
                act(nrm[:], ss[:], AF.Sqrt, r=[('ss', gb, j) for j in range(4)], w=[('nrm', gb)], scale=1.0 / D, bias=EPS)
                sc.op('dve', lambda e: e.reciprocal(out=rstd[:], in_=nrm[:]), r=[('nrm', gb)], w=[('rstd', gb)])
                for j in range(4):
                    sc.op('dve', lambda e: e.tensor_scalar(out=xn[:, j, :], in0=xg[:, j, :], scalar1=rstd[:, j:j + 1],
                                                           scalar2=None, op0=ALU.mult),
                          r=[('xg', gb, j), ('rstd', gb)], w=[('xn', gb, j)])

            def prep_tr(g):
                gb = g % 2
                xn, xnT = xnl[gb], xnTl[gb]
                for c in range(8):
                    ntp[0] += 1
                    pi = ntp[0] % 2
                    p_ = pt[pi]
                    for j in range(4):
                        tr(p_[:, j * 128:(j + 1) * 128], xn[:, j, c * 128:(c + 1) * 128], cB('ident'),
                           r=[('xn', gb, j), 'cb'], w=[('pt', pi)])
                    sc.op('dve', lambda e: e.tensor_scalar(out=xnT[:, c, :], in0=p_[:, :], scalar1=gm[:, c:c + 1],
                                                           scalar2=None, op0=ALU.mult),
                          r=[('pt', pi), 'gm'], w=[('xnT', gb, c)])

            prep_dma(0)
            prep_cmp(0)
            prep_tr(0)
            for g in range(NG):
                t0 = g * 512
                gb = g % 2
                xnT = xnTl[gb]
                if g + 1 < NG:
                    prep_dma(g + 1)

                def proj_fm(c0):
                    p_, pr = next_pj()
                    for kc in range(8):
                        mm(p_[:, :], wb[:, kc, c0:c0 + 128], xnT[:, kc, :], kc == 0, kc == 7,
                           r=[wkey(c0), ('xnT', gb, kc)], w=[pr])
                    return p_, pr

                def qk_tail(cc, p_, pr):
                    isq = cc < 4
                    i2 = cc % 2
                    sq, nr2, rinv, pss = sql[i2], nr2l[i2], rinvl[i2], pssl[i2]
                    mm(pss[:, :], cB('bones'), sq[:], True, True, r=['cb', ('sq', i2)], w=[('pss', i2)])
                    if isq:
                        act(nr2[:], pss[:, :], AF.Ln, r=[('pss', i2)], w=[('nr2', i2)], scale=1.0, bias=64.0 * EPS)
                    else:
                        act(nr2[:], pss[:, :], AF.Ln, r=[('pss', i2)], w=[('nr2', i2)], scale=1.0 / 64.0, bias=EPS)
                    act(rinv[:], nr2[:], AF.Exp, r=[('nr2', i2)], w=[('rinv', i2)], scale=-0.5)
                    o_, orr = next_ob()
                    gcol = gqk[:, 0:1] if isq else gqk[:, 1:2]
                    sc.op('dve', lambda e: e.scalar_tensor_tensor(out=o_[:], in0=p_[:, :], scalar=gcol, in1=rinv[:],
                                                                  op0=ALU.mult, op1=ALU.mult),
                          r=[pr, ('rinv', i2), 'gqk'], w=[orr])
                    dma('sp', qk_att_d[cc * 128:(cc + 1) * 128, t0:t0 + 512], o_[:], r=[orr], w=[uq()])

                prev = None
                for cc in range(8):
                    p_, pr = proj_fm(cc * 128)
                    act(sql[cc % 2][:], p_[:, :], AF.Square, r=[pr], w=[('sq', cc % 2)])
                    if prev is not None:
                        qk_tail(*prev)
                    prev = (cc, p_, pr)
                for cm in range(8):
                    p_, pr = proj_fm(1536 + cm * 128)
                    if cm == 0:
                        qk_tail(*prev)
                    RW = ('raw', cm)
                    act(raw[:, cm, 3:515], p_[:, :], AF.Copy, r=[pr, 'raw'], w=[RW])
                    pc_, pcr = next_pj()
                    for jj in range(4):
                        mm(pc_[:, :], dg[:, cm, jj, :], raw[:, cm, jj:jj + 512], jj == 0, jj == 3, r=['dg', RW], w=[pcr])
                    o_, orr = next_ob()
                    act(o_[:], pc_[:, :], AF.Silu, r=[pcr], w=[orr])
                    dma('sp', qk_ml_d[cm * 128:(cm + 1) * 128, t0:t0 + 512], o_[:], r=[orr], w=[uq()])
                    sc.op('pool', lambda e: e.tensor_copy(out=raw[:, cm, 0:3], in_=raw[:, cm, 512:515]),
                          r=[RW], w=[RW])
                if g + 1 < NG:
                    prep_cmp(g + 1)
                for j in range(4):
                    tk = t0 + j * 128
                    if j == 2 and g + 1 < NG:
                        prep_tr(g + 1)
                    for (c0, n, kind) in [(1024, 512, 'va'), (2560, 512, 'vm'), (3072, 512, 'og'), (3584, 8, 'if')]:
                        p_, pr = next_pj()
                        for kc in range(8):
                            mm(p_[:, 0:n], xnT[:, kc, j * 128:(j + 1) * 128], wb[:, kc, c0:c0 + n], kc == 0, kc == 7,
                               r=[wkey(c0), ('xnT', gb, kc)], w=[pr])
                        if kind == 'if':
                            sc.op('dve', lambda e: e.tensor_tensor(out=gt[:], in0=p_[:, 0:8], in1=bif[:], op=ALU.add),
                                  r=[pr, 'bif'], w=['gt'])
                            dma('sp', gates_d[tk:tk + 128, :], gt[:], r=['gt'], w=[uq()])
                        else:
                            o_, orr = next_ob()
                            if kind == 'vm':
                                sc.op('dve', lambda e: e.tensor_copy(out=o_[:], in_=p_[:, :]), r=[pr], w=[orr])
                            else:
                                act(o_[:], p_[:, :], AF.Sigmoid if kind == 'og' else AF.Copy, r=[pr], w=[orr])
                            dst = {'va': v_att_d, 'vm': v_ml_d, 'og': og_d}[kind]
                            dma('sp', dst[tk:tk + 128, :], o_[:], r=[orr], w=[uq()])

    def phase_B(l):
        NSB = S // 2048
        LAG = 3
        NBUF = LAG + 1
        with ExitStack() as st:
            def sbt(name, shape, dt):
                return st.enter_context(nc.sbuf_tensor(f"B{l}_{name}", list(shape), dt))

            def pst(name, shape, dt=F32):
                return st.enter_context(nc.psum_tensor(f"B{l}_{name}", list(shape), dt))
            qTl = [sbt(f"qT{i}", [64, S], BF16) for i in range(2)]
            kTl = [sbt(f"kT{i}", [64, S], BF16) for i in range(2)]
            vdl = [{d: sbt(f"vd{d}_{i}", [128, S // 128, 128], BF16) for d in (1, 4, 16)} for i in range(2)]
            M4 = sbt("M4", [128, 640], BF16)
            M4c = sbt("M4c", [128, 512], BF16)
            ptl = [sbt(f"pt{i}", [128, 512], BF16) for i in range(NBUF)]
            rden = [sbt(f"rden{i}", [64, 512], F32) for i in range(2)]
            yo = [sbt(f"yo{i}", [64, 512], BF16) for i in range(2)]
            acc = pst("acc", [128, 4, 512])
            stl = [pst(f"st{i}", [128, 512]) for i in range(NBUF)]
            for i in range(5):
                sc.op('dve', lambda e: e.tensor_copy(out=M4[:, i * 128:(i + 1) * 128],
                                                     in_=cB('mprev' if i % 2 == 0 else 'mcur')), r=['cb'], w=['M4'])
            for i in range(4):
                sc.op('dve', lambda e: e.tensor_copy(out=M4c[:, i * 128:(i + 1) * 128], in_=cB('mcur')),
                      r=['cb'], w=['M4c'])
            for i in range(2):
                for d in (1, 4, 16):
                    sc.op('pool', lambda e: e.memset(vdl[i][d][:, :, 64:128], 1.0), w=[('vd', i, d, 'ones')])

            def tok(base, d_):
                return slice(base, base + d_ * 127 + 1, d_)

            def load_head(h):
                hb = h % 2
                qs = ['pool', 'sp'] if h == 0 else ['pool', 'pool']
                dma(qs[0], qTl[hb][:, :], qk_att_d[h * 64:(h + 1) * 64, :], r=[], w=[('qT', hb)])
                dma(qs[1], kTl[hb][:, :], qk_att_d[512 + h * 64:512 + (h + 1) * 64, :], r=[], w=[('kT', hb)])
                vsrc = v_att_d[:, h * 64:(h + 1) * 64]
                k_ = 0
                for d in (16, 4):
                    for r_ in range(d):
                        s_ap = vsrc[r_:S:d, :].rearrange("(n k) c -> k n c", k=128)
                        o_ap = vdl[hb][d][:, r_:S // 128:d, 0:64]
                        dma(qs[k_ % 2], o_ap, s_ap, r=[], w=[('vd', hb, d, r_)])
                        k_ += 1
                for sbi in range(NSB):
                    dma(qs[k_ % 2], vdl[hb][1][:, sbi * 16:(sbi + 1) * 16, 0:64],
                        vsrc[sbi * 2048:(sbi + 1) * 2048, :].rearrange("(n k) c -> k n c", k=128), r=[], w=[('vd', hb, 1, sbi)])
                    k_ += 1

            def head_items():
                items = []
                for sbi in range(NSB):
                    T0 = sbi * 2048

                    def add(pairs):
                        for g0 in range(0, len(pairs), 4):
                            items.append(('g', pairs[g0:g0 + 4], sbi))
                    pairs = []
                    for r_ in range(16):
                        outs = [(seg, slice(r_, r_ + 16 * 31 + 1, 16), slice(32 * seg, 32 * seg + 32)) for seg in range(4)]
                        if sbi > 0:
                            pairs.append((T0 + r_, T0 - 2048 + r_, 16, (sbi - 1) * 16 + r_, 'p', outs))
                        pairs.append((T0 + r_, T0 + r_, 16, sbi * 16 + r_, 'c', outs))
                    add(pairs)
                    for seg in range(4):
                        pairs = []
                        n4 = sbi * 4 + seg
                        for r_ in range(4):
                            outs = [(seg, slice(r_, r_ + 4 * 127 + 1, 4), slice(0, 128))]
                            qb = T0 + seg * 512 + r_
                            if n4 > 0:
                                pairs.append((qb, qb - 512, 4, (n4 - 1) * 4 + r_, 'p', outs))
                            pairs.append((qb, qb, 4, n4 * 4 + r_, 'c', outs))
                        for b in range(4):
                            nb = sbi * 16 + seg * 4 + b
                            outs = [(seg, slice(b * 128, (b + 1) * 128), slice(0, 128))]
                            if nb > 0:
                                pairs.append((nb * 128, (nb - 1) * 128, 1, nb - 1, 'p', outs))
                            pairs.append((nb * 128, nb * 128, 1, nb, 'c', outs))
                        add(pairs)
                        items.append(('n', seg, sbi))
                return items

            ngrp = [0]
            nyo = [0]
            touched = {}

            def stage1(h, grp):
                hb = h % 2
                qT, kT = qTl[hb], kTl[hb]
                ngrp[0] += 1
                bi = ngrp[0] % NBUF
                st_, pt_ = stl[bi], ptl[bi]
                n = len(grp)
                for i, (qb, kb, d_, vb, mt, outs) in enumerate(grp):
                    mm(st_[:, i * 128:(i + 1) * 128], kT[0:64, tok(kb, d_)], qT[0:64, tok(qb, d_)],
                       True, True, r=[('qT', hb), ('kT', hb)], w=[('st', bi)], skip=True)
                act(pt_[:, 0:n * 128], st_[:, 0:n * 128], AF.Exp, r=[('st', bi)], w=[('pt', bi)])
                types = [p[4] for p in grp]
                if all(t == 'c' for t in types):
                    msl = [(0, n, M4c[:, 0:n * 128], 'M4c')]
                elif all(types[i] != types[i + 1] for i in range(n - 1)):
                    o0 = 0 if types[0] == 'p' else 1
                    msl = [(0, n, M4[:, o0 * 128:(o0 + n) * 128], 'M4')]
                else:
                    msl = [(i, 1, M4c[:, 0:128] if types[i] == 'c' else M4[:, 0:128],
                            'M4c' if types[i] == 'c' else 'M4') for i in range(n)]
                for (i0, cnt, map_, mres) in msl:
                    sc.op('dve', lambda e: e.tensor_tensor(out=pt_[:, i0 * 128:(i0 + cnt) * 128],
                                                           in0=pt_[:, i0 * 128:(i0 + cnt) * 128],
                                                           in1=map_, op=ALU.mult),
                          r=[mres, ('pt', bi)], w=[('pt', bi)])
                return bi

            def stage2(h, grp, sbi, bi):
                hb = h % 2
                pt_ = ptl[bi]
                for i, (qb, kb, d_, vb, mt, outs) in enumerate(grp):
                    for (seg, cols, qsub) in outs:
                        key = (h, sbi, seg)
                        first = key not in touched
                        touched[key] = True
                        mm(acc[:, seg, cols], vdl[hb][d_][:, vb, :], pt_[:, i * 128 + qsub.start:i * 128 + qsub.stop],
                           first, True, r=[('vd', hb, d_, 'ones'), ('vd', hb, d_, (vb % d_) if d_ > 1 else (vb // 16)), ('pt', bi)], w=[('acc', seg)], skip=True)

            def norm(h, seg, sbi):
                T0 = sbi * 2048
                nyo[0] += 1
                yi = nyo[0] % 2
                act(rden[yi][:, :], acc[64:128, seg, :], AF.Ln, r=[('acc', seg)], w=[('rden', yi)])
                act(rden[yi][:, :], rden[yi][:, :], AF.Exp, r=[('rden', yi)], w=[('rden', yi)], scale=-1.0)
                sc.op('dve', lambda e: e.tensor_tensor(out=yo[yi][:, :], in0=acc[0:64, seg, :], in1=rden[yi][:, :],
                                                       op=ALU.mult),
                      r=[('acc', seg), ('rden', yi)], w=[('yo', yi)])
                dma('sp', yT_d[h * 64:(h + 1) * 64, T0 + seg * 512:T0 + (seg + 1) * 512], yo[yi][:, :],
                    r=[('yo', yi)], w=[uq()])

            allitems = []
            for h in range(8):
                allitems.append(('load', h))
                for it in head_items():
                    allitems.append((it[0], h) + it[1:])
            order = []
            back = [0]
            npend = [0]

            def drain(k):
                while npend[0] > k:
                    it = order[back[0]]
                    if it[0] == 'g':
                        stage2(it[1], it[2], it[3], it[4])
                        npend[0] -= 1
                    else:
                        norm(it[1], it[2], it[3])
                    back[0] += 1
                while back[0] < len(order) and order[back[0]][0] == 'n':
                    it = order[back[0]]
                    norm(it[1], it[2], it[3])
                    back[0] += 1

            load_head(0)
            cnt_h = {}
            for it in allitems:
                if it[0] == 'load':
                    continue
                if it[0] == 'g':
                    _, h, grp, sbi = it
                    bi = stage1(h, grp)
                    order.append(('g', h, grp, sbi, bi))
                    npend[0] += 1
                    cnt_h[h] = cnt_h.get(h, 0) + 1
                    if cnt_h[h] == LAG + 2 and h + 1 < 8:
                        load_head(h + 1)
                else:
                    order.append(it)
                drain(LAG)
            drain(0)

    def phase_C(l):
        CK = 128
        SEG = 2048
        NSEG = S // SEG
        CPS = SEG // CK
        C0 = -0.5 * math.log(128.0)
        with ExitStack() as st:
            def sbt(name, shape, dt):
                return st.enter_context(nc.sbuf_tensor(f"C{l}_{name}", list(shape), dt))

            def pst(name, shape, dt=F32):
                return st.enter_context(nc.psum_tensor(f"C{l}_{name}", list(shape), dt))
            qcl = [sbt(f"qc{i}", [128, 4, SEG], BF16) for i in range(2)]
            kcl = [sbt(f"kc{i}", [128, 4, SEG], BF16) for i in range(2)]
            vaugl = [sbt(f"vaug{i}", [128, CPS, 4, 132], BF16) for i in range(2)]
            ogl = [sbt(f"og{i}", [128, CPS, 512], BF16) for i in range(2)]
            gtsl = [sbt(f"gts{i}", [128, CPS, 8], F32) for i in range(2)]
            yTs = sbt("yTs", [128, 4, SEG], BF16)
            Sf = sbt("Sf", [128, 4, 132], F32)
            Sb = sbt("Sb", [128, 4, 132], BF16)
            t1 = sbt("t1", [128, CPS, 4], F32)
            sp_ = sbt("sp", [128, CPS, 4], F32)
            t3 = sbt("t3", [128, CPS, 4], F32)
            esl = [sbt(f"es{i}", [128, CPS, 4], F32) for i in range(2)]
            ebl_ = [sbt(f"eb{i}", [128, CPS, 4], F32) for i in range(2)]
            ebll = [sbt(f"ebl{i}", [128, CPS, 4], F32) for i in range(2)]
            ATm = [sbt(f"ATm{i}", [128, 4, 128], BF16) for i in range(2)]
            kp = [sbt(f"kp{i}", [128, 4, 128], BF16) for i in range(2)]
            ychl = [sbt(f"ych{i}", [128, 4, 128], BF16) for i in range(2)]
            ytmpl = [sbt(f"ytmp{i}", [128, 4, 128], F32) for i in range(2)]
            d1 = sbt("d1", [128, 4], F32)
            d2 = sbt("d2", [128, 4], F32)
            d3 = sbt("d3", [128, 4], F32)
            scl = sbt("scl", [128, 4], F32)
            pa = [pst(f"pa{i}", [128, 4, 128]) for i in range(2)]
            pkb = [pst(f"pkb{i}", [128, 8, 128], BF16) for i in range(2)]
            pr = pst("pr", [128, 4, 256])
            pu = pst("pu", [128, 4, 256])
            for i in range(2):
                sc.op('pool', lambda e: e.memset(vaugl[i][:], 1.0), w=[('vaug', i)])
            sc.op('pool', lambda e: e.memset(Sf[:], 0.0), w=['Sf'])
            sc.op('pool', lambda e: e.memset(Sb[:], 0.0), w=['Sb'])

            def dve(fn, r, w):
                sc.op('dve', fn, r=r, w=w)

            def load_seg(sg):
                b = sg % 2
                T0 = sg * SEG
                dma('sp', qcl[b][:], qk_ml_d[0:512, T0:T0 + SEG].rearrange("(h p) t -> p h t", p=128), r=[], w=[('qc', b)])
                dma('sp', kcl[b][:], qk_ml_d[512:1024, T0:T0 + SEG].rearrange("(h p) t -> p h t", p=128), r=[], w=[('kc', b)])
                for h in range(4):
                    dma('pool', vaugl[b][:, :, h, 0:128],
                        v_ml_d[T0:T0 + SEG, h * 128:(h + 1) * 128].rearrange("(c p) f -> p c f", p=128),
                        r=[], w=[('vaug', b)])
                dma('pool', ogl[b][:], og_d[T0:T0 + SEG, :].rearrange("(c p) f -> p c f", p=128), r=[], w=[('og', b)])
                dma('pool', gtsl[b][:], gates_d[T0:T0 + SEG, :].rearrange("(c p) f -> p c f", p=128), r=[], w=[('gts', b)])

            def prepass(sg):
                b = sg % 2
                gts, es_, eb, ebl = gtsl[b], esl[b], ebl_[b], ebll[b]
                act(t1[:], gts[:, :, 4:8], AF.Exp, r=[('gts', b)], w=['t1'], scale=-1.0)
                act(sp_[:], t1[:], AF.Ln, r=['t1'], w=['sp'], bias=1.0)
                spf = sp_[:].rearrange("p c h -> p (c h)")
                NW = CPS * 4
                mm(pu[:, 0, 0:NW], cF('mcur'), spf, True, True, r=['cf', 'sp'], w=['pu'])
                mm(pu[:, 1, 0:NW], cF('ones'), spf, True, True, r=['cf', 'sp'], w=['pu'])
                nb = pu[:, 0, 0:NW].rearrange("p (c h) -> p c h", h=4)
                dve(lambda e: e.tensor_tensor(out=t3[:], in0=nb, in1=gts[:, :, 0:4], op=ALU.add), r=['pu', ('gts', b)], w=['t3'])
                act(es_[:], t3[:], AF.Exp, r=['t3'], w=[('es', b)], bias=C0)
                act(eb[:], nb, AF.Exp, r=['pu'], w=[('eb', b)], scale=-1.0)
                act(ebl[:], pu[:, 1, 0:NW].rearrange("p (c h) -> p c h", h=4), AF.Exp, r=['pu'], w=[('ebl', b)], scale=-1.0)

            NCK = S // CK

            def stage1(ci):
                sg, c = divmod(ci, CPS)
                b = sg % 2
                pb = ci % 2
                lo = c * CK
                qc, kc, es_ = qcl[b], kcl[b], esl[b]
                for h in range(4):
                    mm(pa[pb][:, h, :], kc[:, h, lo:lo + CK], qc[:, h, lo:lo + CK], True, True,
                       r=[('kc', b), ('qc', b)], w=[('pa', pb)])
                for h in range(4):
                    tr(pkb[pb][:, h, :], kc[:, h, lo:lo + CK], cB('ident'), r=[('kc', b), 'cb'], w=[('pkb', pb)])
                esb = es_[:, c, :, None].to_broadcast([128, 4, 128])
                dve(lambda e: e.tensor_tensor(out=ATm[pb][:], in0=pa[pb][:], in1=esb, op=ALU.mult),
                    r=[('pa', pb), ('es', b)], w=[('ATm', pb)])
                sc.op('pool', lambda e: e.tensor_tensor(out=ATm[pb][:], in0=ATm[pb][:],
                                                        in1=cB('mcur')[:, None, :].to_broadcast([128, 4, 128]), op=ALU.mult),
                      r=[('ATm', pb), 'cb'], w=[('ATm', pb)])
                for h in range(4):
                    act(kp[pb][:, h, :], pkb[pb][:, h, :], AF.Copy, r=[('pkb', pb), ('es', b)], w=[('kp', pb)],
                        scale=es_[:, c, h:h + 1])

            def stage2(ci):
                sg, c = divmod(ci, CPS)
                b = sg % 2
                pb = ci % 2
                lo = c * CK
                qc, vaug, og, eb, ebl = qcl[b], vaugl[b], ogl[b], ebl_[b], ebll[b]
                for h in range(4):
                    mm(pr[:, h, 0:129], ATm[pb][:, h, :], vaug[:, c, h, 0:129], True, False,
                       r=[('ATm', pb), ('vaug', b)], w=['pr'])
                    mm(pr[:, h, 0:129], qc[:, h, lo:lo + CK], Sb[:, h, 0:129], False, True, r=[('qc', b), 'Sb'], w=['pr'])
                for h in range(4):
                    mm(pu[:, h, 0:129], kp[pb][:, h, :], vaug[:, c, h, 0:129], True, True, r=[('kp', pb), ('vaug', b)], w=['pu'])
                dve(lambda e: e.tensor_tensor(out=Sf[:, :, 0:129], in0=pu[:, :, 0:129], in1=Sf[:, :, 0:129], op=ALU.add),
                    r=['pu', 'Sf'], w=['Sf'])
                dve(lambda e: e.tensor_tensor(out=Sf[:, :, 0:129], in0=Sf[:, :, 0:129],
                                              in1=ebl[:, c, :, None].to_broadcast([128, 4, 129]), op=ALU.mult),
                    r=[('ebl', b), 'Sf'], w=['Sf'])
                dve(lambda e: e.tensor_tensor(out=d1[:], in0=pr[:, :, 128], in1=eb[:, c, :], op=ALU.mult),
                    r=['pr', ('eb', b)], w=['d1'])
                dve(lambda e: e.tensor_scalar(out=d2[:], in0=d1[:], scalar1=-1.0, scalar2=1.0, op0=ALU.mult, op1=ALU.max),
                    r=['d1'], w=['d2'])
                dve(lambda e: e.tensor_tensor(out=d2[:], in0=d2[:], in1=d1[:], op=ALU.max), r=['d1', 'd2'], w=['d2'])
                dve(lambda e: e.reciprocal(out=d3[:], in_=d2[:]), r=['d2'], w=['d3'])
                dve(lambda e: e.tensor_tensor(out=scl[:], in0=d3[:], in1=eb[:, c, :], op=ALU.mult), r=['d3', ('eb', b)], w=['scl'])
                act(Sb[:, :, 0:129], Sf[:, :, 0:129], AF.Copy, r=['Sf'], w=['Sb'])
                ytmp, ych = ytmpl[pb], ychl[pb]
                dve(lambda e: e.tensor_tensor(out=ytmp[:], in0=pr[:, :, 0:128], in1=scl[:, :, None].to_broadcast([128, 4, 128]),
                                              op=ALU.mult), r=['pr', 'scl'], w=[('ytmp', pb)])
                sc.op('pool', lambda e: e.tensor_tensor(out=ych[:], in0=ytmp[:], in1=og[:, c, :].rearrange("p (h f) -> p h f", h=4),
                                                        op=ALU.mult), r=[('ytmp', pb), ('og', b)], w=[('ych', pb)])

            def stage3(ci):
                sg, c = divmod(ci, CPS)
                pb = ci % 2
                lo = c * CK
                ych = ychl[pb]
                for h in range(4):
                    tr(pkb[pb][:, 4 + h, :], ych[:, h, :], cB('ident'), r=[('ych', pb), 'cb'], w=[('pkb', pb)])
                act(yTs[:, :, lo:lo + CK], pkb[pb][:, 4:8, :], AF.Copy, r=[('pkb', pb)], w=['yTs'])
                if c == CPS - 1:
                    T0 = sg * SEG
                    dma('sp', yT_d[512:1024, T0:T0 + SEG].rearrange("(h p) t -> p h t", p=128), yTs[:], r=['yTs'], w=[uq()])

            load_seg(0)
            prepass(0)
            stage1(0)
            for ci in range(NCK):
                sg, c = divmod(ci, CPS)
                if c == 0 and sg + 1 < NSEG:
                    load_seg(sg + 1)
                if ci + 1 < NCK:
                    if (ci + 1) % CPS == 0:
                        prepass((ci + 1) // CPS)
                    stage1(ci + 1)
                stage2(ci)
                if ci > 0:
                    stage3(ci - 1)
            stage3(NCK - 1)

    BIG = 1.0e4

    def phase_D(l, xin):
        with ExitStack() as st:
            def sbt(name, shape, dt):
                return st.enter_context(nc.sbuf_tensor(f"D{l}_{name}", list(shape), dt))

            def pst(name, shape, dt=F32):
                return st.enter_context(nc.psum_tensor(f"D{l}_{name}", list(shape), dt))
            wo = sbt("wo", [128, 8, D], BF16)
            gf = sbt("gf", [128, D], F32)
            wr = sbt("wr", [128, 8, 36], F32)
            br = sbt("br", [128, 36], F32)
            ebase = sbt("ebase", [128, 32], F32)
            carry = sbt("carry", [128, 32], F32)
            ygl = [sbt(f"yg{i}", [128, 8, 512], BF16) for i in range(2)]
            xgl = [sbt(f"xg{i}", [128, 4, D], F32) for i in range(2)]
            x1l = [sbt(f"x1{i}", [128, 4, D], F32) for i in range(2)]
            junk = sbt("junk", [128, D], BF16)
            s2l = [sbt(f"s2{i}", [128, 12], F32) for i in range(2)]
            xn2f = [sbt(f"xn2f{i}", [128, D], F32) for i in range(2)]
            xn2b = sbt("xn2b", [128, 4, D], BF16)
            xT = [sbt(f"xT{i}", [128, 8, 128], F32) for i in range(2)]
            lg = sbt("lg", [128, 4, 36], F32)
            gmx = sbt("gmx", [128, 4], F32)
            goh = sbt("goh", [128, 4, 4], F32)
            gsub = sbt("gsub", [128, 4, 4], F32)
            ge = sbt("ge", [128, 4, 4], F32)
            gsum = sbt("gsum", [128, 4], F32)
            ggate = sbt("ggate", [128, 4], F32)
            pen = sbt("pen", [128, 4, 4], F32)
            em = sbt("em", [128, 4, 32], F32)
            em2 = sbt("em2", [128, 4, 32], F32)
            m1 = sbt("m1", [128, 4], F32)
            m2 = sbt("m2", [128, 4], F32)
            oh1 = sbt("oh1", [128, 4, 32], F32)
            oh2 = sbt("oh2", [128, 4, 32], F32)
            dm = sbt("dm", [128, 4], F32)
            ex = sbt("ex", [128, 4], F32)
            p1 = sbt("p1", [128, 4], F32)
            aa = sbt("aa", [128, 4, 32], F32)
            slot = sbt("slot", [128, 4, 32], F32)
            crun = sbt("crun", [128, 32], F32)
            tmp = sbt("tmp", [128, 4, 32], F32)
            dstf = sbt("dstf", [128, 4, 2], F32)
            pj = [pst(f"pj{i}", [128, 512]) for i in range(4)]
            ptf = [pst(f"ptf{i}", [128, 512]) for i in range(2)]
            plg = pst("plg", [128, 4, 64])
            ppos = pst("ppos", [128, 256])
            dma('pool', wo[:], w_out_d[l].rearrange("(kc p) n -> p kc n", p=128), r=[], w=['wo'])
            dma('sp', gf[:], g_ffn_d[l].partition_broadcast(128), r=[], w=['gf'])
            dma('sp', wr[:], w_r_d[l].rearrange("(kc p) n -> p kc n", p=128), r=[], w=['wr'])
            dma('sp', br[:], b_r_d[l].partition_broadcast(128), r=[], w=['br'])
            dma('sp', ebase[:], ebase_d[:, :], r=[], w=['ebase'])
            sc.op('pool', lambda e: e.memset(carry[:], 0.0), w=['carry'])

            def dve(fn, r, w):
                sc.op('dve', fn, r=r, w=w)

            def load(g):
                t0 = g * 512
                dma('sp', ygl[g % 2][:], yT_d[:, t0:t0 + 512].rearrange("(c p) t -> p c t", p=128), r=[], w=[('yg', g % 2)])
                dma('sp', xgl[g % 2][:], xin[t0:t0 + 512, :].rearrange("(j p) f -> p j f", p=128), r=[], w=[('xg', g % 2)])
            load(0)
            npj = [0]

            def outproj(g):
                t0 = g * 512
                gb = g % 2
                yg, xg, x1, s2 = ygl[gb], xgl[gb], x1l[gb], s2l[gb]
                if g + 1 < NG:
                    load(g + 1)
                for j in range(4):
                    tk = t0 + j * 128
                    for hf in range(2):
                        npj[0] += 1
                        pi = npj[0] % 4
                        p_ = pj[pi]
                        for kc in range(8):
                            mm(p_[:, :], yg[:, kc, j * 128:(j + 1) * 128], wo[:, kc, hf * 512:(hf + 1) * 512], kc == 0, kc == 7,
                               r=[('yg', gb), 'wo'], w=[('pj', pi)])
                        dve(lambda e: e.tensor_tensor(out=x1[:, j, hf * 512:(hf + 1) * 512], in0=p_[:, :],
                                                      in1=xg[:, j, hf * 512:(hf + 1) * 512], op=ALU.add),
                            r=[('pj', pi), ('xg', gb)], w=[('x1', gb, j)])
                    dma('sp', out_d[tk:tk + 128, :], x1[:, j, :], r=[('x1', gb, j)], w=[('out_d', g * 4 + j)])
                    act(junk[:], x1[:, j, :], AF.Square, r=[('x1', gb, j)], w=['junk', ('s2a', gb, j)], accum_out=s2[:, j:j + 1])
                act(s2[:, 4:8], s2[:, 0:4], AF.Sqrt, r=[('s2a', gb, j) for j in range(4)], w=[('s2b', gb)], scale=1.0 / D, bias=EPS)
                dve(lambda e: e.reciprocal(out=s2[:, 8:12], in_=s2[:, 4:8]), r=[('s2b', gb)], w=[('s2c', gb)])

            outproj(0)
            for g in range(NG):
                t0 = g * 512
                gb = g % 2
                x1, s2 = x1l[gb], s2l[gb]
                if g + 1 < NG:
                    outproj(g + 1)
                for j in range(4):
                    xf = xn2f[j % 2]
                    XF = ('xn2f', j % 2)
                    dve(lambda e: e.scalar_tensor_tensor(out=xf[:], in0=x1[:, j, :], scalar=s2[:, 8 + j:9 + j], in1=gf[:],
                                                         op0=ALU.mult, op1=ALU.mult), r=[('x1', gb, j), ('s2c', gb), 'gf'], w=[XF])
                    act(xn2b[:, j, :], xf[:], AF.Copy, r=[XF], w=[('xn2b', j)])
                    for c in range(8):
                        tr(ptf[c // 4][:, (c % 4) * 128:(c % 4 + 1) * 128], xf[:, c * 128:(c + 1) * 128], cF('ident'),
                           r=[XF, 'cf'], w=[('ptf', c // 4)])
                    for hf in range(2):
                        act(xT[j % 2][:, hf * 4:(hf + 1) * 4, :], ptf[hf][:, :].rearrange("p (c t) -> p c t", t=128), AF.Copy,
                            r=[('ptf', hf)], w=[('xT', j % 2)])
                    for c in range(8):
                        mm(plg[:, j, 0:36], xT[j % 2][:, c, :], wr[:, c, :], c == 0, c == 7, r=[('xT', j % 2), 'wr'], w=['plg'])
                dve(lambda e: e.tensor_tensor(out=lg[:], in0=plg[:, :, 0:36], in1=br[:, None, :].to_broadcast([128, 4, 36]),
                                              op=ALU.add), r=['plg', 'br'], w=['lg'])
                dve(lambda e: e.tensor_reduce(out=gmx[:], in_=lg[:, :, 0:4], axis=AX.X, op=ALU.max), r=['lg'], w=['gmx'])
                dve(lambda e: e.tensor_tensor(out=goh[:], in0=lg[:, :, 0:4], in1=gmx[:, :, None].to_broadcast([128, 4, 4]),
                                              op=ALU.is_equal), r=['lg', 'gmx'], w=['goh'])
                dve(lambda e: e.tensor_tensor(out=gsub[:], in0=lg[:, :, 0:4], in1=gmx[:, :, None].to_broadcast([128, 4, 4]),
                                              op=ALU.subtract), r=['lg', 'gmx'], w=['gsub'])
                act(ge[:], gsub[:], AF.Exp, r=['gsub'], w=['ge'])
                dve(lambda e: e.tensor_reduce(out=gsum[:], in_=ge[:], axis=AX.X, op=ALU.add), r=['ge'], w=['gsum'])
                dve(lambda e: e.reciprocal(out=ggate[:], in_=gsum[:]), r=['gsum'], w=['ggate'])
                dve(lambda e: e.tensor_scalar(out=pen[:], in0=goh[:], scalar1=BIG, scalar2=-BIG, op0=ALU.mult, op1=ALU.add),
                    r=['goh'], w=['pen'])
                dve(lambda e: e.tensor_tensor(out=em[:].rearrange("p j (g e) -> p j g e", e=8),
                                              in0=lg[:, :, 4:36].rearrange("p j (g e) -> p j g e", e=8),
                                              in1=pen[:, :, :, None].to_broadcast([128, 4, 4, 8]), op=ALU.add),
                    r=['lg', 'pen'], w=['em'])
                dve(lambda e: e.tensor_reduce(out=m1[:], in_=em[:], axis=AX.X, op=ALU.max), r=['em'], w=['m1'])
                dve(lambda e: e.tensor_tensor(out=oh1[:], in0=em[:], in1=m1[:, :, None].to_broadcast([128, 4, 32]),
                                              op=ALU.is_equal), r=['em', 'm1'], w=['oh1'])
                dve(lambda e: e.scalar_tensor_tensor(out=em2[:], in0=oh1[:], scalar=-BIG, in1=em[:], op0=ALU.mult, op1=ALU.add),
                    r=['oh1', 'em'], w=['em2'])
                dve(lambda e: e.tensor_reduce(out=m2[:], in_=em2[:], axis=AX.X, op=ALU.max), r=['em2'], w=['m2'])
                dve(lambda e: e.tensor_tensor(out=oh2[:], in0=em2[:], in1=m2[:, :, None].to_broadcast([128, 4, 32]),
                                              op=ALU.is_equal), r=['em2', 'm2'], w=['oh2'])
                dve(lambda e: e.tensor_tensor(out=dm[:], in0=m2[:], in1=m1[:], op=ALU.subtract), r=['m1', 'm2'], w=['dm'])
                act(ex[:], dm[:], AF.Exp, r=['dm'], w=['ex'])
                dve(lambda e: e.tensor_scalar(out=p1[:], in0=ex[:], scalar1=1.0, scalar2=None, op0=ALU.add), r=['ex'], w=['p1'])
                dve(lambda e: e.reciprocal(out=p1[:], in_=p1[:]), r=['p1'], w=['p1'])
                dve(lambda e: e.tensor_tensor(out=RT[:, g * 4:g * 4 + 4, 0], in0=p1[:], in1=ggate[:], op=ALU.mult),
                    r=['p1', 'ggate'], w=['RT'])
                dve(lambda e: e.tensor_tensor(out=RT[:, g * 4:g * 4 + 4, 1], in0=RT[:, g * 4:g * 4 + 4, 0], in1=ex[:], op=ALU.mult),
                    r=['ex', 'RT'], w=['RT'])
                dve(lambda e: e.tensor_tensor(out=aa[:], in0=oh1[:], in1=oh2[:], op=ALU.add), r=['oh1', 'oh2'], w=['aa'])
                aaf = aa[:].rearrange("p j e -> p (j e)")
                mm(ppos[:, 0:128], cF('ustrict'), aaf, True, True, r=['cf', 'aa'], w=['ppos'])
                mm(ppos[:, 128:256], cF('ones'), aaf, True, True, r=['cf', 'aa'], w=['ppos'])
                for j in range(4):
                    dve(lambda e: e.tensor_tensor(out=slot[:, j, :], in0=ppos[:, j * 32:(j + 1) * 32], in1=carry[:], op=ALU.add),
                        r=['ppos', 'carry'], w=[('slot', j)])
                    dve(lambda e: e.tensor_tensor(out=carry[:], in0=ppos[:, 128 + j * 32:128 + (j + 1) * 32], in1=carry[:],
                                                  op=ALU.add), r=['ppos', 'carry'], w=['carry'])
                SL = [('slot', j) for j in range(4)]
                dve(lambda e: e.tensor_scalar(out=slot[:], in0=slot[:], scalar1=float(CAP - 1), scalar2=None, op0=ALU.min),
                    r=SL, w=SL)
                dve(lambda e: e.tensor_tensor(out=slot[:], in0=slot[:], in1=ebase[:, None, :].to_broadcast([128, 4, 32]), op=ALU.add),
                    r=SL + ['ebase'], w=SL)
                for k_, oh in enumerate((oh1, oh2)):
                    dve(lambda e: e.tensor_tensor(out=tmp[:], in0=slot[:], in1=oh[:], op=ALU.mult),
                        r=SL + ['oh1', 'oh2'], w=['tmp'])
                    dve(lambda e: e.tensor_reduce(out=dstf[:, :, k_], in_=tmp[:], axis=AX.X, op=ALU.add), r=['tmp'], w=['dstf'])
                dve(lambda e: e.tensor_copy(out=RTi[:, g * 4:g * 4 + 4, :], in_=dstf[:]), r=['dstf'], w=['RTi'])
                for j in range(4):
                    for k_ in range(2):
                        sc.dma('pool', lambda e: e.indirect_dma_start(
                            out=xs_d[:, :], out_offset=bass.IndirectOffsetOnAxis(ap=RTi[:, g * 4 + j, k_:k_ + 1], axis=0),
                            in_=xn2b[:, j, :], in_offset=None), r=['RTi', ('xn2b', j)], w=[uq()])

    def phase_F(l):
        GS = 384
        NGR = CAP // GS
        NB = GS // 128
        with ExitStack() as st:
            def sbt(name, shape, dt):
                return st.enter_context(nc.sbuf_tensor(f"F{l}_{name}", list(shape), dt))

            def pst(name, shape, dt=F32):
                return st.enter_context(nc.psum_tensor(f"F{l}_{name}", list(shape), dt))
            w1b = [sbt(f"w1b{i}", [128, 8, DEXP], BF16) for i in range(2)]
            w3b = [sbt(f"w3b{i}", [128, 8, DEXP], BF16) for i in range(2)]
            w2b = [sbt(f"w2b{i}", [128, 4, D], BF16) for i in range(2)]
            xsg = [sbt(f"xsg{i}", [128, NB, D], BF16) for i in range(2)]
            xT = [sbt(f"xT{i}", [128, 8, GS], BF16) for i in range(2)]
            s1 = [sbt(f"s1{i}", [128, GS], F32) for i in range(2)]
            hT = [sbt(f"hT{i}", [128, 4, GS], BF16) for i in range(2)]
            yb = [sbt(f"yb{i}", [128, D], BF16) for i in range(4)]
            ptx = [pst(f"ptx{i}", [128, 8, 128], BF16) for i in range(2)]
            ph1 = [pst(f"ph1{i}", [128, 512]) for i in range(2)]
            ph3 = [pst(f"ph3{i}", [128, 512]) for i in range(2)]
            pyy = [pst(f"pyy{i}", [128, 512]) for i in range(2)]
            nyb = [0]
            ntx = [0]
            nh = [0]
            gidx = 0
            groups = [(ex, gi) for ex in range(NEXP) for gi in range(NGR)]

            def load_w(ex):
                wi = ex % 2
                dma('pool', w1b[wi][:], w1_d[l, ex].rearrange("(kc p) n -> p kc n", p=128), r=[], w=[('w1b', wi)])
                dma('pool', w3b[wi][:], w3_d[l, ex].rearrange("(kc p) n -> p kc n", p=128), r=[], w=[('w3b', wi)])
                dma('pool', w2b[wi][:], w2_d[l, ex].rearrange("(kc p) n -> p kc n", p=128), r=[], w=[('w2b', wi)])

            def load_x(k):
                ex, gi = groups[k]
                r0 = ex * CAP + gi * GS
                dma('sp', xsg[k % 2][:], xs_d[r0:r0 + GS, :].rearrange("(b p) f -> p b f", p=128), r=[], w=[('xsg', k % 2)])

            load_w(0)
            load_x(0)
            for k, (ex, gi) in enumerate(groups):
                wi = ex % 2
                gb = k % 2
                r0 = ex * CAP + gi * GS
                if gi == 0 and ex + 1 < NEXP:
                    load_w(ex + 1)
                if k + 1 < len(groups):
                    load_x(k + 1)
                for b in range(NB):
                    ntx[0] += 1
                    ti = ntx[0] % 2
                    for c in range(8):
                        tr(ptx[ti][:, c, :], xsg[gb][:, b, c * 128:(c + 1) * 128], cB('ident'), r=[('xsg', gb), 'cb'], w=[('ptx', ti)])
                    if b % 2 == 0:
                        act(xT[gb][:, :, b * 128:(b + 1) * 128], ptx[ti][:, :, :], AF.Copy, r=[('ptx', ti)], w=[('xT', gb)])
                    else:
                        sc.op('dve', lambda e: e.tensor_copy(out=xT[gb][:, :, b * 128:(b + 1) * 128], in_=ptx[ti][:, :, :]),
                              r=[('ptx', ti)], w=[('xT', gb)])
                for m_ in range(4):
                    nh[0] += 1
                    hi = nh[0] % 2
                    for kc in range(8):
                        mm(ph1[hi][:, 0:GS], w1b[wi][:, kc, m_ * 128:(m_ + 1) * 128], xT[gb][:, kc, :], kc == 0, kc == 7,
                           r=[('w1b', wi), ('xT', gb)], w=[('ph1', hi)])
                    for kc in range(8):
                        mm(ph3[hi][:, 0:GS], w3b[wi][:, kc, m_ * 128:(m_ + 1) * 128], xT[gb][:, kc, :], kc == 0, kc == 7,
                           r=[('w3b', wi), ('xT', gb)], w=[('ph3', hi)])
                    act(s1[hi][:], ph1[hi][:, 0:GS], AF.Silu, r=[('ph1', hi)], w=[('s1', hi)])
                    sc.op('dve', lambda e: e.tensor_tensor(out=hT[gb][:, m_, :], in0=ph3[hi][:, 0:GS], in1=s1[hi][:], op=ALU.mult),
                          r=[('ph3', hi), ('s1', hi)], w=[('hT', gb, m_)])
                for b in range(NB):
                    nyb[0] += 1
                    yi = nyb[0] % 4
                    for hf in range(2):
                        for m_ in range(4):
                            mm(pyy[hf][:, :], hT[gb][:, m_, b * 128:(b + 1) * 128], w2b[wi][:, m_, hf * 512:(hf + 1) * 512],
                               m_ == 0, m_ == 3, r=[('hT', gb, m_), ('w2b', wi)], w=[('pyy', hf)])
                        if hf == 0:
                            act(yb[yi][:, 0:512], pyy[hf][:, :], AF.Copy, r=[('pyy', hf)], w=[('yb', yi)])
                        else:
                            sc.op('dve', lambda e: e.tensor_copy(out=yb[yi][:, 512:1024], in_=pyy[hf][:, :]),
                                  r=[('pyy', hf)], w=[('yb', yi)])
                    dma('sp', ys_d[r0 + b * 128:r0 + (b + 1) * 128, :], yb[yi][:], r=[('yb', yi)], w=[uq()])

    def phase_G(l):
        with ExitStack() as st:
            def sbt(name, shape, dt):
                return st.enter_context(nc.sbuf_tensor(f"G{l}_{name}", list(shape), dt))
            r1 = [sbt(f"r1{i}", [128, D], BF16) for i in range(4)]
            r2 = [sbt(f"r2{i}", [128, D], BF16) for i in range(4)]
            xx = [sbt(f"xx{i}", [128, D], F32) for i in range(4)]
            for ti in range(NT):
                i = ti % 4
                tk = ti * 128
                sc.dma('pool', lambda e: e.indirect_dma_start(
                    out=r1[i][:, :], out_offset=None, in_=ys_d[:, :],
                    in_offset=bass.IndirectOffsetOnAxis(ap=RTi[:, ti, 0:1], axis=0)), r=['RTi'], w=[('r1', i)])
                sc.dma('pool', lambda e: e.indirect_dma_start(
                    out=r2[i][:, :], out_offset=None, in_=ys_d[:, :],
                    in_offset=bass.IndirectOffsetOnAxis(ap=RTi[:, ti, 1:2], axis=0)), r=['RTi'], w=[('r2', i)])
                dma('act', xx[i][:], out_d[tk:tk + 128, :], r=[], w=[('xx', i)])
                sc.op('dve', lambda e: e.scalar_tensor_tensor(out=xx[i][:], in0=r1[i][:], scalar=RT[:, ti, 0:1], in1=xx[i][:],
                                                              op0=ALU.mult, op1=ALU.add), r=[('r1', i), 'RT', ('xx', i)], w=[('xx', i)])
                sc.op('dve', lambda e: e.scalar_tensor_tensor(out=xx[i][:], in0=r2[i][:], scalar=RT[:, ti, 1:2], in1=xx[i][:],
                                                              op0=ALU.mult, op1=ALU.add), r=[('r2', i), 'RT', ('xx', i)], w=[('xx', i)])
                dma('sp', out_d[tk:tk + 128, :], xx[i][:], r=[('xx', i)], w=[('out_d', ti)])

    xin = x_in
    for l in range(L):
        if 'A' in phases:
            phase_A(l, xin, fuse_g=(l > 0 and 'G' in phases))
            sc.barrier()
        if 'B' in phases:
            if l == 0 and 'D' in phases:
                sc.op('pool', lambda e: e.memset(zt[:], 0.0), w=['zt'])
                sc._wait('act', [sc.lastw['zt']])
                rows = NEXP * CAP
                sc.dma_bulk('act', [
                    (lambda e, r0=r0: e.dma_start(out=xs_d[r0:r0 + 512, :].rearrange("(p k) f -> p (k f)", k=4), in_=zt[:]))
                    for r0 in range(0, rows, 512)])
            phase_B(l)
            sc.barrier()
        if 'C' in phases:
            phase_C(l)
            sc.barrier()
        if 'D' in phases:
            phase_D(l, xin)
            sc.barrier()
        if 'F' in phases:
            phase_F(l)
            sc.barrier()
        if 'G' in phases and l == L - 1:
            phase_G(l)
            sc.barrier()
        xin = out_d

    final = ['out_d', 'qk_att_d', 'qk_ml_d', 'va_d', 'vm_d', 'og_d', 'gates_d', 'yT_d']
    sc.finish(final)
    es.close()
    return nc, sc


def host_layout(inp, L, CAP=768):
    f = lambda a: np.ascontiguousarray(np.asarray(a, dtype=np.float32))
    m = {}
    m['consts'] = host_consts()
    m['ebase'] = np.tile((np.arange(32, dtype=np.float32) * CAP)[None, :], (128, 1))
    m['g_mix_fm'] = f(inp['g_norm_mix'][:L].reshape(L, 8, 128).transpose(0, 2, 1))
    m['w_in'] = f(inp['w_in'][:L])
    m['b_if'] = f(np.concatenate([inp['b_igate'][:L], inp['b_fgate'][:L]], axis=1))
    m['conv_fm'] = f(inp['conv_w'][:L].reshape(L, 4, 8, 128).transpose(0, 3, 2, 1).reshape(L, 128, 32))
    gq = np.tile(inp['g_q'][:L], (1, 2))[:, :, None]
    gk = np.tile(inp['g_k'][:L], (1, 2))[:, :, None]
    m['gqk2'] = f(np.concatenate([gq, gk], axis=2))
    m['w_out'] = f(inp['w_out'][:L])
    m['g_ffn'] = f(inp['g_norm_ffn'][:L])
    m['w_r'] = f(np.concatenate([inp['w_group'][:L], inp['w_expert_router'][:L]], axis=2))
    m['b_r'] = f(np.concatenate([inp['b_group'][:L], inp['b_expert_router'][:L]], axis=1))
    m['w1'] = f(inp['w1'][:L])
    m['w3'] = f(inp['w3'][:L])
    m['w2'] = f(inp['w2'][:L])
    return m


SEQ = 8192
DEPTH = 4
BATCH = 4
CAPACITY = 768


def kernel(**inputs):
    inp = {k: np.asarray(v) for k, v in inputs.items()}
    B, S, _ = inp['x'].shape
    L = inp['w_in'].shape[0]
    nc, _sc = build(S, L, dbg=False, CAP=CAPACITY)
    shared = host_layout(inp, L, CAPACITY)
    in_maps = []
    for b in range(B):
        m = dict(shared)
        m['x'] = np.ascontiguousarray(inp['x'][b], dtype=np.float32)
        in_maps.append(m)
    res = run_bass_kernel_spmd(nc, in_maps, core_ids=list(range(B)))
    out = np.stack([np.asarray(r['out'], dtype=np.float32) for r in res.results], axis=0)
    return out
```

```python
import math
from contextlib import ExitStack
import numpy as np
import concourse.bass as bass
import concourse.mybir as mybir
from concourse.bass_utils import run_bass_kernel_spmd

F32 = mybir.dt.float32
BF16 = mybir.dt.bfloat16
I32 = mybir.dt.int32
U32 = mybir.dt.uint32
AF = mybir.ActivationFunctionType
ALU = mybir.AluOpType
AX = mybir.AxisListType

D = 1024
DIN = 3592
NEXP = 32
DEXP = 512
EPS = 1e-6
SEM_LIM = 30000


class Sched:
    def __init__(self, nc):
        self.nc = nc
        self.eng = {'pe': nc.tensor, 'act': nc.scalar, 'dve': nc.vector, 'pool': nc.gpsimd, 'sp': nc.sync}
        self.cnt = {e: 0 for e in self.eng}
        self.sems = {e: [] for e in self.eng}
        self.seen = {e: {} for e in self.eng}
        self.lastw = {}
        self.readers = {}
        self.dma_sems = {}
        self.dma_rr = {}
        self.nwaits = 0
        self.ninst = 0

    def _sem(self, e, n):
        k = (n - 1) // SEM_LIM
        while len(self.sems[e]) <= k:
            self.sems[e].append(self.nc.alloc_semaphore(f"s_{e}_{len(self.sems[e])}"))
        return self.sems[e][k], (n - 1) % SEM_LIM + 1

    def _wait(self, e, evs):
        need = {}
        for ev in evs:
            if ev is None:
                continue
            if ev[0] == 'e':
                _, e2, n = ev
                if e2 == 'pe' and e == 'pe':
                    continue
                key = ('e', e2)
            else:
                _, si, n = ev
                key = ('d', si)
            if n > need.get(key, 0):
                need[key] = n
        for key, n in need.items():
            if self.seen[e].get(key, 0) >= n:
                continue
            if key[0] == 'e':
                sem, val = self._sem(key[1], n)
            else:
                sem, val = self.dma_sem_handles[key[1]], n
            self.eng[e].wait_ge(sem, val)
            self.nwaits += 1
            self.seen[e][key] = n

    def _deps(self, r, w):
        evs = []
        for res in r:
            evs.append(self.lastw.get(res))
        for res in w:
            evs.append(self.lastw.get(res))
            evs.extend(self.readers.get(res, {}).values())
        return evs

    def _commit(self, ev, r, w):
        for res in w:
            self.lastw[res] = ev
            self.readers[res] = {}
        for res in r:
            if res in w:
                continue
            self.readers.setdefault(res, {})[ev[:2]] = ev

    def op(self, e, build, r=(), w=()):
        self._wait(e, self._deps(r, w))
        ins = build(self.eng[e])
        n = self.cnt[e] + 1
        self.cnt[e] = n
        sem, _ = self._sem(e, n)
        ins.then_inc(sem, 1)
        self.ninst += 1
        self._commit(('e', e, n), r, w)

    dma_sem_handles = None

    def dma(self, q, build, r=(), w=()):
        if self.dma_sem_handles is None:
            self.dma_sem_handles = []
            self.dma_val = []
        pool = self.dma_sems.setdefault(q, [])
        if len(pool) < 8:
            si = len(self.dma_sem_handles)
            self.dma_sem_handles.append(self.nc.alloc_semaphore(f"d_{q}_{len(pool)}"))
            self.dma_val.append(0)
            pool.append(si)
            self.dma_rr[q] = len(pool) - 1
        else:
            self.dma_rr[q] = (self.dma_rr[q] + 1) % len(pool)
            si = pool[self.dma_rr[q]]
        evs = self._deps(r, w)
        if self.dma_val[si] > 0:
            evs.append(('d', si, self.dma_val[si]))
        self._wait(q, evs)
        ins = build(self.eng[q])
        self.dma_val[si] += 16
        ins.then_inc(self.dma_sem_handles[si], 16)
        self.ninst += 1
        self._commit(('d', si, self.dma_val[si]), r, w)

    def dma_bulk(self, q, builders):
        if self.dma_sem_handles is None:
            self.dma_sem_handles = []
            self.dma_val = []
        si = len(self.dma_sem_handles)
        self.dma_sem_handles.append(self.nc.alloc_semaphore(f"dbulk_{si}"))
        self.dma_val.append(0)
        for b in builders:
            ins = b(self.eng[q])
            self.dma_val[si] += 16
            ins.then_inc(self.dma_sem_handles[si], 16)
            self.ninst += 1

    def barrier(self):
        evs = []
        for e in self.eng:
            if self.cnt[e] > 0:
                evs.append(('e', e, self.cnt[e]))
        for si, v in enumerate(self.dma_val or []):
            if v > 0:
                evs.append(('d', si, v))
        for e in self.eng:
            self._wait(e, evs)

    def finish(self, res_list):
        evs = [self.lastw.get(r) for r in res_list]
        for e in self.eng:
            if self.cnt[e] > 0:
                evs.append(('e', e, self.cnt[e]))
        for si, v in enumerate(self.dma_val or []):
            if v > 0:
                evs.append(('d', si, v))
        self._wait('sp', evs)
        self.eng['sp'].nop() if hasattr(self.eng['sp'], 'nop') else None


def host_consts():
    k = np.arange(128)[:, None]
    q = np.arange(128)[None, :]
    c = {}
    c['ident'] = (k == q)
    c['mcur'] = (k <= q)
    c['mprev'] = (k >= q)
    c['bones'] = (k // 64 == q // 64)
    c['ustrict'] = (k < q)
    c['ones'] = np.ones((128, 128), bool)
    return np.concatenate([c[n].astype(np.float32) for n in
                           ['ident', 'mcur', 'mprev', 'bones', 'ustrict', 'ones']], axis=1)


CI = {n: i * 128 for i, n in enumerate(['ident', 'mcur', 'mprev', 'bones', 'ustrict', 'ones'])}
NCONST = 6 * 128


def build(S, L, dbg=False, CAP=768, phases="ABCDEFG"):
    nc = bass.Bass("TRN2", target_bir_lowering=False)
    sc = Sched(nc)
    NT = S // 128
    NG = S // 512
    NCH = S // 64
    skind = "ExternalOutput" if dbg else "Internal"

    def din(name, shape, dt=F32):
        return nc.dram_tensor(name, list(shape), dt, kind="ExternalInput").ap()

    def dsc(name, shape, dt):
        return nc.dram_tensor(name, list(shape), dt, kind=skind).ap()

    x_in = din("x", [S, D])
    consts_d = din("consts", [128, NCONST])
    g_mix_d = din("g_mix_fm", [L, 128, 8])
    w_in_d = din("w_in", [L, D, DIN])
    b_if_d = din("b_if", [L, 8])
    conv_d = din("conv_fm", [L, 128, 8 * 4])
    gqk_d = din("gqk2", [L, 128, 2])
    w_out_d = din("w_out", [L, D, D])
    g_ffn_d = din("g_ffn", [L, D])
    w_r_d = din("w_r", [L, D, 36])
    b_r_d = din("b_r", [L, 36])
    w1_d = din("w1", [L, NEXP, D, DEXP])
    w3_d = din("w3", [L, NEXP, D, DEXP])
    w2_d = din("w2", [L, NEXP, DEXP, D])
    ebase_d = din("ebase", [128, 32])
    out_d = nc.dram_tensor("out", [S, D], F32, kind="ExternalOutput").ap()

    qk_att_d = dsc("qk_att", [1024, S], BF16)
    qk_ml_d = dsc("qk_ml", [1024, S], BF16)
    v_att_d = dsc("v_att", [S, 512], BF16)
    v_ml_d = dsc("v_ml", [S, 512], BF16)
    og_d = dsc("og", [S, 512], BF16)
    gates_d = dsc("gates", [S, 8], F32)
    yT_d = dsc("yT", [1024, S], BF16)
    xs_d = dsc("xs", [NEXP * CAP, D], BF16)
    ys_d = dsc("ys", [NEXP * CAP, D], BF16)

    es = ExitStack()

    def sb(name, shape, dt):
        return es.enter_context(nc.sbuf_tensor(name, list(shape), dt))

    def ps(name, shape, dt=F32):
        return es.enter_context(nc.psum_tensor(name, list(shape), dt))

    cf = sb("cf", [128, NCONST], F32)
    cb = sb("cb", [128, NCONST], BF16)
    sc.dma('sp', lambda e: e.dma_start(out=cf[:], in_=consts_d[:, :]), w=['cf'])
    sc.op('dve', lambda e: e.tensor_copy(out=cb[:], in_=cf[:]), r=['cf'], w=['cb'])

    zt = sb("zt", [128, 4 * D], BF16)
    bg_jobs = []
    RT = sb("RT", [128, NT, 2], F32)
    RTi = sb("RTi", [128, NT, 2], I32)

    def cF(n, p=128, q=128):
        return cf[0:p, CI[n]:CI[n] + q]

    def cB(n, p=128, q=128):
        return cb[0:p, CI[n]:CI[n] + q]

    def mm(out, lhsT, rhs, start, stop, r, w, skip=False):
        sc.op('pe', lambda e: e.matmul(out, lhsT, rhs, start=start, stop=stop, skip_group_check=skip), r=r, w=w)

    def tr(out, in_, ident, r, w):
        sc.op('pe', lambda e: e.transpose(out, in_, ident), r=r, w=w)

    def act(out, in_, func, r, w, **kw):
        sc.op('act', lambda e: e.activation(out=out, in_=in_, func=func, **kw), r=r, w=w)

    def dma(q, out, in_, r, w, **kw):
        sc.dma(q, lambda e: e.dma_start(out=out, in_=in_, **kw), r=r, w=w)

    _uq = [0]

    def uq():
        _uq[0] += 1
        return ('u', _uq[0])

    def phase_A(l, xin, fuse_g=False):
        with ExitStack() as st:
            def sbt(name, shape, dt):
                return st.enter_context(nc.sbuf_tensor(f"A{l}_{name}", list(shape), dt))

            def pst(name, shape, dt=F32):
                return st.enter_context(nc.psum_tensor(f"A{l}_{name}", list(shape), dt))
            wb = sbt("wb", [128, 8, DIN], BF16)
            gm = sbt("gm", [128, 8], F32)
            cw = sbt("cw", [128, 32], F32)
            gqk = sbt("gqk", [128, 2], F32)
            bif = sbt("bif", [128, 8], F32)
            xgl = [sbt(f"xg{i}", [128, 4, D], F32) for i in range(2)]
            junk = sbt("junk", [128, D], BF16)
            ssl = [sbt(f"ss{i}", [128, 4], F32) for i in range(2)]
            nrml = [sbt(f"nrm{i}", [128, 4], F32) for i in range(2)]
            rstdl = [sbt(f"rstd{i}", [128, 4], F32) for i in range(2)]
            xnl = [sbt(f"xn{i}", [128, 4, D], BF16) for i in range(2)]
            xnTl = [sbt(f"xnT{i}", [128, 8, 512], BF16) for i in range(2)]
            sql = [sbt(f"sq{i}", [128, 512], BF16) for i in range(2)]
            nr2l = [sbt(f"nr2{i}", [128, 512], F32) for i in range(2)]
            rinvl = [sbt(f"rinv{i}", [128, 512], F32) for i in range(2)]
            NOB = 8
            ob = [sbt(f"ob{i}", [128, 512], BF16) for i in range(NOB)]
            raw = sbt("raw", [128, 8, 515], BF16)
            dg = sbt("dg", [128, 8, 4, 128], BF16)
            gt = sbt("gt", [128, 8], F32)
            if fuse_g:
                r1l = [sbt(f"r1{i}", [128, D], BF16) for i in range(4)]
                r2l = [sbt(f"r2{i}", [128, D], BF16) for i in range(4)]
            pt = [pst(f"pt{i}", [128, 512], BF16) for i in range(2)]
            pj = [pst(f"pj{i}", [128, 512]) for i in range(3)]
            pssl = [pst(f"pss{i}", [128, 512]) for i in range(2)]
            w_src = w_in_d[l].rearrange("(kc p) n -> p kc n", p=128)
            wpieces = [(0, 512), (512, 1024), (1536, 2048), (2048, 2560), (1024, 1536), (2560, 3072), (3072, DIN)]
            for i_, (a_, b_) in enumerate(wpieces):
                dma('pool', wb[:, :, a_:b_], w_src[:, :, a_:b_], r=[], w=[('wb', i_)])

            def wkey(c0):
                for i_, (a_, b_) in enumerate(wpieces):
                    if a_ <= c0 < b_:
                        return ('wb', i_)
            dma('sp', gm[:], g_mix_d[l], r=[], w=['gm'])
            dma('sp', cw[:], conv_d[l], r=[], w=['cw'])
            dma('sp', gqk[:], gqk_d[l], r=[], w=['gqk'])
            dma('sp', bif[:], b_if_d[l].partition_broadcast(128), r=[], w=['bif'])
            sc.op('pool', lambda e: e.memset(raw[:], 0.0), w=['raw'])
            for cm in range(8):
                for jj in range(4):
                    sc.op('dve', lambda e: e.tensor_scalar(out=dg[:, cm, jj, :], in0=cB('ident'),
                                                           scalar1=cw[:, cm * 4 + jj:cm * 4 + jj + 1], scalar2=None,
                                                           op0=ALU.mult), r=['cb', 'cw'], w=['dg'])
            nob = [0]
            npj = [0]
            ntp = [0]

            def next_pj():
                npj[0] += 1
                i = npj[0] % 3
                return pj[i], ('pj', i)

            def next_ob():
                nob[0] += 1
                i = nob[0] % NOB
                return ob[i], ('ob', i)

            def prep_dma(g):
                gb = g % 2
                t0 = g * 512
                dma('pool', xgl[gb][:], xin[t0:t0 + 512, :].rearrange("(j p) f -> p j f", p=128), r=[], w=[('xg', gb, j_) for j_ in range(4)])
                if fuse_g:
                    for j in range(4):
                        ti = g * 4 + j
                        for (rl, k_, nm) in ((r1l, 0, 'r1'), (r2l, 1, 'r2')):
                            sc.dma('pool', lambda e: e.indirect_dma_start(
                                out=rl[ti % 4][:, :], out_offset=None, in_=ys_d[:, :],
                                in_offset=bass.IndirectOffsetOnAxis(ap=RTi[:, ti, k_:k_ + 1], axis=0)),
                                r=['RTi'], w=[(nm, ti % 4)])

            def prep_cmp(g):
                gb = g % 2
                t0 = g * 512
                xg, ss, nrm, rstd, xn, xnT = xgl[gb], ssl[gb], nrml[gb], rstdl[gb], xnl[gb], xnTl[gb]
                if fuse_g:
                    for j in range(4):
                        ti = g * 4 + j
                        for (rl, k_, nm) in ((r1l, 0, 'r1'), (r2l, 1, 'r2')):
                            sc.op('dve', lambda e: e.scalar_tensor_tensor(out=xg[:, j, :], in0=rl[ti % 4][:], scalar=RT[:, ti, k_:k_ + 1],
                                                                          in1=xg[:, j, :], op0=ALU.mult, op1=ALU.add),
                                  r=[(nm, ti % 4), 'RT', ('xg', gb, j)], w=[('xg', gb, j)])
                        dma('sp', out_d[t0 + j * 128:t0 + (j + 1) * 128, :], xg[:, j, :], r=[('xg', gb, j)], w=[uq()])
                for j in range(4):
                    act(junk[:], xg[:, j, :], AF.Square, r=[('xg', gb, j)], w=['junk', ('ss', gb, j)], accum_out=ss[:, j:j + 1])
                act(nrm[:], ss[:], AF.Sqrt, r=[('ss', gb, j) for j in range(4)], w=[('nrm', gb)], scale=1.0 / D, bias=EPS)
                sc.op('dve', lambda e: e.reciprocal(out=rstd[:], in_=nrm[:]), r=[('nrm', gb)], w=[('rstd', gb)])
                for j in range(4):
                    sc.op('dve', lambda e: e.tensor_scalar(out=xn[:, j, :], in0=xg[:, j, :], scalar1=rstd[:, j:j + 1],
                                                           scalar2=None, op0=ALU.mult),
                          r=[('xg', gb, j), ('rstd', gb)], w=[('xn', gb, j)])

            def prep_tr(g):
                gb = g % 2
                xn, xnT = xnl[gb], xnTl[gb]
                for c in range(8):
                    ntp[0] += 1
                    pi = ntp[0] % 2
                    p_ = pt[pi]
                    for j in range(4):
                        tr(p_[:, j * 128:(j + 1) * 128], xn[:, j, c * 128:(c + 1) * 128], cB('ident'),
                           r=[('xn', gb, j), 'cb'], w=[('pt', pi)])
                    sc.op('dve', lambda e: e.tensor_scalar(out=xnT[:, c, :], in0=p_[:, :], scalar1=gm[:, c:c + 1],
                                                           scalar2=None, op0=ALU.mult),
                          r=[('pt', pi), 'gm'], w=[('xnT', gb, c)])

            prep_dma(0)
            prep_cmp(0)
            prep_tr(0)
            for g in range(NG):
                t0 = g * 512
                gb = g % 2
                xnT = xnTl[gb]
                if g + 1 < NG:
                    prep_dma(g + 1)

                def proj_fm(c0):
                    p_, pr = next_pj()
                    for kc in range(8):
                        mm(p_[:, :], wb[:, kc, c0:c0 + 128], xnT[:, kc, :], kc == 0, kc == 7,
                           r=[wkey(c0), ('xnT', gb, kc)], w=[pr])
                    return p_, pr

                def qk_tail(cc, p_, pr):
                    isq = cc < 4
                    i2 = cc % 2
                    sq, nr2, rinv, pss = sql[i2], nr2l[i2], rinvl[i2], pssl[i2]
                    mm(pss[:, :], cB('bones'), sq[:], True, True, r=['cb', ('sq', i2)], w=[('pss', i2)])
                    if isq:
                        act(nr2[:], pss[:, :], AF.Ln, r=[('pss', i2)], w=[('nr2', i2)], scale=1.0, bias=64.0 * EPS)
                    else:
                        act(nr2[:], pss[:, :], AF.Ln, r=[('pss', i2)], w=[('nr2', i2)], scale=1.0 / 64.0, bias=EPS)
                    act(rinv[:], nr2[:], AF.Exp, r=[('nr2', i2)], w=[('rinv', i2)], scale=-0.5)
                    o_, orr = next_ob()
                    gcol = gqk[:, 0:1] if isq else gqk[:, 1:2]
                    sc.op('dve', lambda e: e.scalar_tensor_tensor(out=o_[:], in0=p_[:, :], scalar=gcol, in1=rinv[:],
                                                                  op0=ALU.mult, op1=ALU.mult),
                          r=[pr, ('rinv', i2), 'gqk'], w=[orr])
                    dma('sp', qk_att_d[cc * 128:(cc + 1) * 128, t0:t0 + 512], o_[:], r=[orr], w=[uq()])

                prev = None
                for cc in range(8):
                    p_, pr = proj_fm(cc * 128)
                    act(sql[cc % 2][:], p_[:, :], AF.Square, r=[pr], w=[('sq', cc % 2)])
                    if prev is not None:
                        qk_tail(*prev)
                    prev = (cc, p_, pr)
                for cm in range(8):
                    p_, pr = proj_fm(1536 + cm * 128)
                    if cm == 0:
                        qk_tail(*prev)
                    RW = ('raw', cm)
                    act(raw[:, cm, 3:515], p_[:, :], AF.Copy, r=[pr, 'raw'], w=[RW])
                    pc_, pcr = next_pj()
                    for jj in range(4):
                        mm(pc_[:, :], dg[:, cm, jj, :], raw[:, cm, jj:jj + 512], jj == 0, jj == 3, r=['dg', RW], w=[pcr])
                    o_, orr = next_ob()
                    act(o_[:], pc_[:, :], AF.Silu, r=[pcr], w=[orr])
                    dma('sp', qk_ml_d[cm * 128:(cm + 1) * 128, t0:t0 + 512], o_[:], r=[orr], w=[uq()])
                    sc.op('pool', lambda e: e.tensor_copy(out=raw[:, cm, 0:3], in_=raw[:, cm, 512:515]),
                          r=[RW], w=[RW])
                if g + 1 < NG:
                    prep_cmp(g + 1)
                for j in range(4):
                    tk = t0 + j * 128
                    if j == 2 and g + 1 < NG:
                        prep_tr(g + 1)
                    for (c0, n, kind) in [(1024, 512, 'va'), (2560, 512, 'vm'), (3072, 512, 'og'), (3584, 8, 'if')]:
                        p_, pr = next_pj()
                        for kc in range(8):
                            mm(p_[:, 0:n], xnT[:, kc, j * 128:(j + 1) * 128], wb[:, kc, c0:c0 + n], kc == 0, kc == 7,
                               r=[wkey(c0), ('xnT', gb, kc)], w=[pr])
                        if kind == 'if':
                            sc.op('dve', lambda e: e.tensor_tensor(out=gt[:], in0=p_[:, 0:8], in1=bif[:], op=ALU.add),
                                  r=[pr, 'bif'], w=['gt'])
                            dma('sp', gates_d[tk:tk + 128, :], gt[:], r=['gt'], w=[uq()])
                        else:
                            o_, orr = next_ob()
                            if kind == 'vm':
                                sc.op('dve', lambda e: e.tensor_copy(out=o_[:], in_=p_[:, :]), r=[pr], w=[orr])
                            else:
                                act(o_[:], p_[:, :], AF.Sigmoid if kind == 'og' else AF.Copy, r=[pr], w=[orr])
                            dst = {'va': v_att_d, 'vm': v_ml_d, 'og': og_d}[kind]
                            dma('sp', dst[tk:tk + 128, :], o_[:], r=[orr], w=[uq()])

    def phase_B(l):
        NSB = S // 2048
        LAG = 3
        NBUF = LAG + 1
        with ExitStack() as st:
            def sbt(name, shape, dt):
                return st.enter_context(nc.sbuf_tensor(f"B{l}_{name}", list(shape), dt))

            def pst(name, shape, dt=F32):
                return st.enter_context(nc.psum_tensor(f"B{l}_{name}", list(shape), dt))
            qTl = [sbt(f"qT{i}", [64, S], BF16) for i in range(2)]
            kTl = [sbt(f"kT{i}", [64, S], BF16) for i in range(2)]
            vdl = [{d: sbt(f"vd{d}_{i}", [128, S // 128, 128], BF16) for d in (1, 4, 16)} for i in range(2)]
            M4 = sbt("M4", [128, 640], BF16)
            M4c = sbt("M4c", [128, 512], BF16)
            ptl = [sbt(f"pt{i}", [128, 512], BF16) for i in range(NBUF)]
            rden = [sbt(f"rden{i}", [64, 512], F32) for i in range(2)]
            yo = [sbt(f"yo{i}", [64, 512], BF16) for i in range(2)]
            acc = pst("acc", [128, 4, 512])
            stl = [pst(f"st{i}", [128, 512]) for i in range(NBUF)]
            for i in range(5):
                sc.op('dve', lambda e: e.tensor_copy(out=M4[:, i * 128:(i + 1) * 128],
                                                     in_=cB('mprev' if i % 2 == 0 else 'mcur')), r=['cb'], w=['M4'])
            for i in range(4):
                sc.op('dve', lambda e: e.tensor_copy(out=M4c[:, i * 128:(i + 1) * 128], in_=cB('mcur')),
                      r=['cb'], w=['M4c'])
            for i in range(2):
                for d in (1, 4, 16):
                    sc.op('pool', lambda e: e.memset(vdl[i][d][:, :, 64:128], 1.0), w=[('vd', i, d, 'ones')])

            def tok(base, d_):
                return slice(base, base + d_ * 127 + 1, d_)

            def load_head(h):
                hb = h % 2
                qs = ['pool', 'sp'] if h == 0 else ['pool', 'pool']
                dma(qs[0], qTl[hb][:, :], qk_att_d[h * 64:(h + 1) * 64, :], r=[], w=[('qT', hb)])
                dma(qs[1], kTl[hb][:, :], qk_att_d[512 + h * 64:512 + (h + 1) * 64, :], r=[], w=[('kT', hb)])
                vsrc = v_att_d[:, h * 64:(h + 1) * 64]
                k_ = 0
                for d in (16, 4):
                    for r_ in range(d):
                        s_ap = vsrc[r_:S:d, :].rearrange("(n k) c -> k n c", k=128)
                        o_ap = vdl[hb][d][:, r_:S // 128:d, 0:64]
                        dma(qs[k_ % 2], o_ap, s_ap, r=[], w=[('vd', hb, d, r_)])
                        k_ += 1
                for sbi in range(NSB):
                    dma(qs[k_ % 2], vdl[hb][1][:, sbi * 16:(sbi + 1) * 16, 0:64],
                        vsrc[sbi * 2048:(sbi + 1) * 2048, :].rearrange("(n k) c -> k n c", k=128), r=[], w=[('vd', hb, 1, sbi)])
                    k_ += 1

            def head_items():
                items = []
                for sbi in range(NSB):
                    T0 = sbi * 2048

                    def add(pairs):
                        for g0 in range(0, len(pairs), 4):
                            items.append(('g', pairs[g0:g0 + 4], sbi))
                    pairs = []
                    for r_ in range(16):
                        outs = [(seg, slice(r_, r_ + 16 * 31 + 1, 16), slice(32 * seg, 32 * seg + 32)) for seg in range(4)]
                        if sbi > 0:
                            pairs.append((T0 + r_, T0 - 2048 + r_, 16, (sbi - 1) * 16 + r_, 'p', outs))
                        pairs.append((T0 + r_, T0 + r_, 16, sbi * 16 + r_, 'c', outs))
                    add(pairs)
                    for seg in range(4):
                        pairs = []
                        n4 = sbi * 4 + seg
                        for r_ in range(4):
                            outs = [(seg, slice(r_, r_ + 4 * 127 + 1, 4), slice(0, 128))]
                            qb = T0 + seg * 512 + r_
                            if n4 > 0:
                                pairs.append((qb, qb - 512, 4, (n4 - 1) * 4 + r_, 'p', outs))
                            pairs.append((qb, qb, 4, n4 * 4 + r_, 'c', outs))
                        for b in range(4):
                            nb = sbi * 16 + seg * 4 + b
                            outs = [(seg, slice(b * 128, (b + 1) * 128), slice(0, 128))]
                            if nb > 0:
                                pairs.append((nb * 128, (nb - 1) * 128, 1, nb - 1, 'p', outs))
                            pairs.append((nb * 128, nb * 128, 1, nb, 'c', outs))
                        add(pairs)
                        items.append(('n', seg, sbi))
                return items

            ngrp = [0]
            nyo = [0]
            touched = {}

            def stage1(h, grp):
                hb = h % 2
                qT, kT = qTl[hb], kTl[hb]
                ngrp[0] += 1
                bi = ngrp[0] % NBUF
                st_, pt_ = stl[bi], ptl[bi]
                n = len(grp)
                for i, (qb, kb, d_, vb, mt, outs) in enumerate(grp):
                    mm(st_[:, i * 128:(i + 1) * 128], kT[0:64, tok(kb, d_)], qT[0:64, tok(qb, d_)],
                       True, True, r=[('qT', hb), ('kT', hb)], w=[('st', bi)], skip=True)
                act(pt_[:, 0:n * 128], st_[:, 0:n * 128], AF.Exp, r=[('st', bi)], w=[('pt', bi)])
                types = [p[4] for p in grp]
                if all(t == 'c' for t in types):
                    msl = [(0, n, M4c[:, 0:n * 128], 'M4c')]
                elif all(types[i] != types[i + 1] for i in range(n - 1)):
                    o0 = 0 if types[0] == 'p' else 1
                    msl = [(0, n, M4[:, o0 * 128:(o0 + n) * 128], 'M4')]
                else:
                    msl = [(i, 1, M4c[:, 0:128] if types[i] == 'c' else M4[:, 0:128],
                            'M4c' if types[i] == 'c' else 'M4') for i in range(n)]
                for (i0, cnt, map_, mres) in msl:
                    sc.op('dve', lambda e: e.tensor_tensor(out=pt_[:, i0 * 128:(i0 + cnt) * 128],
                                                           in0=pt_[:, i0 * 128:(i0 + cnt) * 128],
                                                           in1=map_, op=ALU.mult),
                          r=[mres, ('pt', bi)], w=[('pt', bi)])
                return bi

            def stage2(h, grp, sbi, bi):
                hb = h % 2
                pt_ = ptl[bi]
                for i, (qb, kb, d_, vb, mt, outs) in enumerate(grp):
                    for (seg, cols, qsub) in outs:
                        key = (h, sbi, seg)
                        first = key not in touched
                        touched[key] = True
                        mm(acc[:, seg, cols], vdl[hb][d_][:, vb, :], pt_[:, i * 128 + qsub.start:i * 128 + qsub.stop],
                           first, True, r=[('vd', hb, d_, 'ones'), ('vd', hb, d_, (vb % d_) if d_ > 1 else (vb // 16)), ('pt', bi)], w=[('acc', seg)], skip=True)

            def norm(h, seg, sbi):
                T0 = sbi * 2048
                nyo[0] += 1
                yi = nyo[0] % 2
                act(rden[yi][:, :], acc[64:128, seg, :], AF.Ln, r=[('acc', seg)], w=[('rden', yi)])
                act(rden[yi][:, :], rden[yi][:, :], AF.Exp, r=[('rden', yi)], w=[('rden', yi)], scale=-1.0)
                sc.op('dve', lambda e: e.tensor_tensor(out=yo[yi][:, :], in0=acc[0:64, seg, :], in1=rden[yi][:, :],
                                                       op=ALU.mult),
                      r=[('acc', seg), ('rden', yi)], w=[('yo', yi)])
                dma('sp', yT_d[h * 64:(h + 1) * 64, T0 + seg * 512:T0 + (seg + 1) * 512], yo[yi][:, :],
                    r=[('yo', yi)], w=[uq()])

            allitems = []
            for h in range(8):
                allitems.append(('load', h))
                for it in head_items():
                    allitems.append((it[0], h) + it[1:])
            order = []
            back = [0]
            npend = [0]

            def drain(k):
                while npend[0] > k:
                    it = order[back[0]]
                    if it[0] == 'g':
                        stage2(it[1], it[2], it[3], it[4])
                        npend[0] -= 1
                    else:
                        norm(it[1], it[2], it[3])
                    back[0] += 1
                while back[0] < len(order) and order[back[0]][0] == 'n':
                    it = order[back[0]]
                    norm(it[1], it[2], it[3])
                    back[0] += 1

            load_head(0)
            cnt_h = {}
            for it in allitems:
                if it[0] == 'load':
                    continue
                if it[0] == 'g':
                    _, h, grp, sbi = it
                    bi = stage1(h, grp)
                    order.append(('g', h, grp, sbi, bi))
                    npend[0] += 1
                    cnt_h[h] = cnt_h.get(h, 0) + 1
                    if cnt_h[h] == LAG + 2 and h + 1 < 8:
                        load_head(h + 1)
                        if bg_jobs:
                            nj = (len(bg_jobs) + (7 - h) - 1) // max(1, 7 - h)
                            sc.dma_bulk('act', bg_jobs[:nj])
                            del bg_jobs[:nj]
                else:
                    order.append(it)
                drain(LAG)
            drain(0)

    def phase_C(l):
        CK = 128
        SEG = 2048
        NSEG = S // SEG
        CPS = SEG // CK
        C0 = -0.5 * math.log(128.0)
        with ExitStack() as st:
            def sbt(name, shape, dt):
                return st.enter_context(nc.sbuf_tensor(f"C{l}_{name}", list(shape), dt))

            def pst(name, shape, dt=F32):
                return st.enter_context(nc.psum_tensor(f"C{l}_{name}", list(shape), dt))
            qcl = [sbt(f"qc{i}", [128, 4, SEG], BF16) for i in range(2)]
            kcl = [sbt(f"kc{i}", [128, 4, SEG], BF16) for i in range(2)]
            vaugl = [sbt(f"vaug{i}", [128, CPS, 4, 132], BF16) for i in range(2)]
            ogl = [sbt(f"og{i}", [128, CPS, 512], BF16) for i in range(2)]
            gtsl = [sbt(f"gts{i}", [128, CPS, 8], F32) for i in range(2)]
            yTs = sbt("yTs", [128, 4, SEG], BF16)
            Sf = sbt("Sf", [128, 4, 132], F32)
            Sb = sbt("Sb", [128, 4, 132], BF16)
            t1 = sbt("t1", [128, CPS, 4], F32)
            sp_ = sbt("sp", [128, CPS, 4], F32)
            t3 = sbt("t3", [128, CPS, 4], F32)
            esl = [sbt(f"es{i}", [128, CPS, 4], F32) for i in range(2)]
            ebl_ = [sbt(f"eb{i}", [128, CPS, 4], F32) for i in range(2)]
            ebll = [sbt(f"ebl{i}", [128, CPS, 4], F32) for i in range(2)]
            ATm = [sbt(f"ATm{i}", [128, 4, 128], BF16) for i in range(2)]
            kp = [sbt(f"kp{i}", [128, 4, 128], BF16) for i in range(2)]
            ychl = [sbt(f"ych{i}", [128, 4, 128], BF16) for i in range(2)]
            ytmpl = [sbt(f"ytmp{i}", [128, 4, 128], F32) for i in range(2)]
            d1 = sbt("d1", [128, 4], F32)
            d2 = sbt("d2", [128, 4], F32)
            d3 = sbt("d3", [128, 4], F32)
            scl = sbt("scl", [128, 4], F32)
            pa = [pst(f"pa{i}", [128, 4, 128]) for i in range(2)]
            pkb = [pst(f"pkb{i}", [128, 8, 128], BF16) for i in range(2)]
            pr = pst("pr", [128, 4, 256])
            pu = pst("pu", [128, 4, 256])
            for i in range(2):
                sc.op('pool', lambda e: e.memset(vaugl[i][:], 1.0), w=[('vaug', i)])
            sc.op('pool', lambda e: e.memset(Sf[:], 0.0), w=['Sf'])
            sc.op('pool', lambda e: e.memset(Sb[:], 0.0), w=['Sb'])

            def dve(fn, r, w):
                sc.op('dve', fn, r=r, w=w)

            def load_seg(sg):
                b = sg % 2
                T0 = sg * SEG
                dma('sp', qcl[b][:], qk_ml_d[0:512, T0:T0 + SEG].rearrange("(h p) t -> p h t", p=128), r=[], w=[('qc', b)])
                dma('sp', kcl[b][:], qk_ml_d[512:1024, T0:T0 + SEG].rearrange("(h p) t -> p h t", p=128), r=[], w=[('kc', b)])
                for h in range(4):
                    dma('pool', vaugl[b][:, :, h, 0:128],
                        v_ml_d[T0:T0 + SEG, h * 128:(h + 1) * 128].rearrange("(c p) f -> p c f", p=128),
                        r=[], w=[('vaug', b)])
                dma('pool', ogl[b][:], og_d[T0:T0 + SEG, :].rearrange("(c p) f -> p c f", p=128), r=[], w=[('og', b)])
                dma('pool', gtsl[b][:], gates_d[T0:T0 + SEG, :].rearrange("(c p) f -> p c f", p=128), r=[], w=[('gts', b)])

            def prepass(sg):
                b = sg % 2
                gts, es_, eb, ebl = gtsl[b], esl[b], ebl_[b], ebll[b]
                act(t1[:], gts[:, :, 4:8], AF.Exp, r=[('gts', b)], w=['t1'], scale=-1.0)
                act(sp_[:], t1[:], AF.Ln, r=['t1'], w=['sp'], bias=1.0)
                spf = sp_[:].rearrange("p c h -> p (c h)")
                NW = CPS * 4
                mm(pu[:, 0, 0:NW], cF('mcur'), spf, True, True, r=['cf', 'sp'], w=['pu'])
                mm(pu[:, 1, 0:NW], cF('ones'), spf, True, True, r=['cf', 'sp'], w=['pu'])
                nb = pu[:, 0, 0:NW].rearrange("p (c h) -> p c h", h=4)
                dve(lambda e: e.tensor_tensor(out=t3[:], in0=nb, in1=gts[:, :, 0:4], op=ALU.add), r=['pu', ('gts', b)], w=['t3'])
                act(es_[:], t3[:], AF.Exp, r=['t3'], w=[('es', b)], bias=C0)
                act(eb[:], nb, AF.Exp, r=['pu'], w=[('eb', b)], scale=-1.0)
                act(ebl[:], pu[:, 1, 0:NW].rearrange("p (c h) -> p c h", h=4), AF.Exp, r=['pu'], w=[('ebl', b)], scale=-1.0)

            NCK = S // CK

            def stage1(ci):
                sg, c = divmod(ci, CPS)
                b = sg % 2
                pb = ci % 2
                lo = c * CK
                qc, kc, es_ = qcl[b], kcl[b], esl[b]
                for h in range(4):
                    mm(pa[pb][:, h, :], kc[:, h, lo:lo + CK], qc[:, h, lo:lo + CK], True, True,
                       r=[('kc', b), ('qc', b)], w=[('pa', pb)])
                for h in range(4):
                    tr(pkb[pb][:, h, :], kc[:, h, lo:lo + CK], cB('ident'), r=[('kc', b), 'cb'], w=[('pkb', pb)])
                esb = es_[:, c, :, None].to_broadcast([128, 4, 128])
                dve(lambda e: e.tensor_tensor(out=ATm[pb][:], in0=pa[pb][:], in1=esb, op=ALU.mult),
                    r=[('pa', pb), ('es', b)], w=[('ATm', pb)])
                sc.op('pool', lambda e: e.tensor_tensor(out=ATm[pb][:], in0=ATm[pb][:],
                                                        in1=cB('mcur')[:, None, :].to_broadcast([128, 4, 128]), op=ALU.mult),
                      r=[('ATm', pb), 'cb'], w=[('ATm', pb)])
                for h in range(4):
                    act(kp[pb][:, h, :], pkb[pb][:, h, :], AF.Copy, r=[('pkb', pb), ('es', b)], w=[('kp', pb)],
                        scale=es_[:, c, h:h + 1])

            def stage2(ci):
                sg, c = divmod(ci, CPS)
                b = sg % 2
                pb = ci % 2
                lo = c * CK
                qc, vaug, og, eb, ebl = qcl[b], vaugl[b], ogl[b], ebl_[b], ebll[b]
                for h in range(4):
                    mm(pr[:, h, 0:129], ATm[pb][:, h, :], vaug[:, c, h, 0:129], True, False,
                       r=[('ATm', pb), ('vaug', b)], w=['pr'])
                    mm(pr[:, h, 0:129], qc[:, h, lo:lo + CK], Sb[:, h, 0:129], False, True, r=[('qc', b), 'Sb'], w=['pr'])
                for h in range(4):
                    mm(pu[:, h, 0:129], kp[pb][:, h, :], vaug[:, c, h, 0:129], True, True, r=[('kp', pb), ('vaug', b)], w=['pu'])
                dve(lambda e: e.tensor_tensor(out=Sf[:, :, 0:129], in0=pu[:, :, 0:129], in1=Sf[:, :, 0:129], op=ALU.add),
                    r=['pu', 'Sf'], w=['Sf'])
                dve(lambda e: e.tensor_tensor(out=Sf[:, :, 0:129], in0=Sf[:, :, 0:129],
                                              in1=ebl[:, c, :, None].to_broadcast([128, 4, 129]), op=ALU.mult),
                    r=[('ebl', b), 'Sf'], w=['Sf'])
                dve(lambda e: e.tensor_tensor(out=d1[:], in0=pr[:, :, 128], in1=eb[:, c, :], op=ALU.mult),
                    r=['pr', ('eb', b)], w=['d1'])
                dve(lambda e: e.tensor_scalar(out=d2[:], in0=d1[:], scalar1=-1.0, scalar2=1.0, op0=ALU.mult, op1=ALU.max),
                    r=['d1'], w=['d2'])
                dve(lambda e: e.tensor_tensor(out=d2[:], in0=d2[:], in1=d1[:], op=ALU.max), r=['d1', 'd2'], w=['d2'])
                dve(lambda e: e.reciprocal(out=d3[:], in_=d2[:]), r=['d2'], w=['d3'])
                dve(lambda e: e.tensor_tensor(out=scl[:], in0=d3[:], in1=eb[:, c, :], op=ALU.mult), r=['d3', ('eb', b)], w=['scl'])
                act(Sb[:, :, 0:129], Sf[:, :, 0:129], AF.Copy, r=['Sf'], w=['Sb'])
                ytmp, ych = ytmpl[pb], ychl[pb]
                dve(lambda e: e.tensor_tensor(out=ytmp[:], in0=pr[:, :, 0:128], in1=scl[:, :, None].to_broadcast([128, 4, 128]),
                                              op=ALU.mult), r=['pr', 'scl'], w=[('ytmp', pb)])
                sc.op('pool', lambda e: e.tensor_tensor(out=ych[:], in0=ytmp[:], in1=og[:, c, :].rearrange("p (h f) -> p h f", h=4),
                                                        op=ALU.mult), r=[('ytmp', pb), ('og', b)], w=[('ych', pb)])

            def stage3(ci):
                sg, c = divmod(ci, CPS)
                pb = ci % 2
                lo = c * CK
                ych = ychl[pb]
                for h in range(4):
                    tr(pkb[pb][:, 4 + h, :], ych[:, h, :], cB('ident'), r=[('ych', pb), 'cb'], w=[('pkb', pb)])
                act(yTs[:, :, lo:lo + CK], pkb[pb][:, 4:8, :], AF.Copy, r=[('pkb', pb)], w=['yTs'])
                if c == CPS - 1:
                    T0 = sg * SEG
                    dma('sp', yT_d[512:1024, T0:T0 + SEG].rearrange("(h p) t -> p h t", p=128), yTs[:], r=['yTs'], w=[uq()])

            load_seg(0)
            prepass(0)
            stage1(0)
            for ci in range(NCK):
                sg, c = divmod(ci, CPS)
                if c == 0 and sg + 1 < NSEG:
                    load_seg(sg + 1)
                if ci + 1 < NCK:
                    if (ci + 1) % CPS == 0:
                        prepass((ci + 1) // CPS)
                    stage1(ci + 1)
                stage2(ci)
                if ci > 0:
                    stage3(ci - 1)
            stage3(NCK - 1)

    BIG = 1.0e4

    def phase_D(l, xin):
        with ExitStack() as st:
            def sbt(name, shape, dt):
                return st.enter_context(nc.sbuf_tensor(f"D{l}_{name}", list(shape), dt))

            def pst(name, shape, dt=F32):
                return st.enter_context(nc.psum_tensor(f"D{l}_{name}", list(shape), dt))
            wo = sbt("wo", [128, 8, D], BF16)
            gf = sbt("gf", [128, D], F32)
            wr = sbt("wr", [128, 8, 36], F32)
            br = sbt("br", [128, 36], F32)
            ebase = sbt("ebase", [128, 32], F32)
            carry = sbt("carry", [128, 32], F32)
            ygl = [sbt(f"yg{i}", [128, 8, 512], BF16) for i in range(2)]
            xgl = [sbt(f"xg{i}", [128, 4, D], F32) for i in range(2)]
            x1l = [sbt(f"x1{i}", [128, 4, D], F32) for i in range(2)]
            junk = sbt("junk", [128, D], BF16)
            s2l = [sbt(f"s2{i}", [128, 12], F32) for i in range(2)]
            xn2f = [sbt(f"xn2f{i}", [128, D], F32) for i in range(2)]
            xn2b = sbt("xn2b", [128, 4, D], BF16)
            xT = [sbt(f"xT{i}", [128, 8, 128], F32) for i in range(2)]
            lg = sbt("lg", [128, 4, 36], F32)
            gmx = sbt("gmx", [128, 4], F32)
            goh = sbt("goh", [128, 4, 4], F32)
            gsub = sbt("gsub", [128, 4, 4], F32)
            ge = sbt("ge", [128, 4, 4], F32)
            gsum = sbt("gsum", [128, 4], F32)
            ggate = sbt("ggate", [128, 4], F32)
            pen = sbt("pen", [128, 4, 4], F32)
            em = sbt("em", [128, 4, 32], F32)
            em2 = sbt("em2", [128, 4, 32], F32)
            m1 = sbt("m1", [128, 4], F32)
            m2 = sbt("m2", [128, 4], F32)
            oh1 = sbt("oh1", [128, 4, 32], F32)
            oh2 = sbt("oh2", [128, 4, 32], F32)
            dm = sbt("dm", [128, 4], F32)
            ex = sbt("ex", [128, 4], F32)
            p1 = sbt("p1", [128, 4], F32)
            aa = sbt("aa", [128, 4, 32], F32)
            slot = sbt("slot", [128, 4, 32], F32)
            crun = sbt("crun", [128, 32], F32)
            tmp = sbt("tmp", [128, 4, 32], F32)
            dstf = sbt("dstf", [128, 4, 2], F32)
            pj = [pst(f"pj{i}", [128, 512]) for i in range(4)]
            ptf = [pst(f"ptf{i}", [128, 512]) for i in range(2)]
            plg = pst("plg", [128, 4, 64])
            ppos = pst("ppos", [128, 256])
            dma('pool', wo[:], w_out_d[l].rearrange("(kc p) n -> p kc n", p=128), r=[], w=['wo'])
            dma('sp', gf[:], g_ffn_d[l].partition_broadcast(128), r=[], w=['gf'])
            dma('sp', wr[:], w_r_d[l].rearrange("(kc p) n -> p kc n", p=128), r=[], w=['wr'])
            dma('sp', br[:], b_r_d[l].partition_broadcast(128), r=[], w=['br'])
            dma('sp', ebase[:], ebase_d[:, :], r=[], w=['ebase'])
            sc.op('pool', lambda e: e.memset(carry[:], 0.0), w=['carry'])

            def dve(fn, r, w):
                sc.op('dve', fn, r=r, w=w)

            def load(g):
                t0 = g * 512
                dma('sp', ygl[g % 2][:], yT_d[:, t0:t0 + 512].rearrange("(c p) t -> p c t", p=128), r=[], w=[('yg', g % 2)])
                dma('sp', xgl[g % 2][:], xin[t0:t0 + 512, :].rearrange("(j p) f -> p j f", p=128), r=[], w=[('xg', g % 2)])
            load(0)
            npj = [0]

            def outproj(g):
                t0 = g * 512
                gb = g % 2
                yg, xg, x1, s2 = ygl[gb], xgl[gb], x1l[gb], s2l[gb]
                if g + 1 < NG:
                    load(g + 1)
                for j in range(4):
                    tk = t0 + j * 128
                    for hf in range(2):
                        npj[0] += 1
                        pi = npj[0] % 4
                        p_ = pj[pi]
                        for kc in range(8):
                            mm(p_[:, :], yg[:, kc, j * 128:(j + 1) * 128], wo[:, kc, hf * 512:(hf + 1) * 512], kc == 0, kc == 7,
                               r=[('yg', gb), 'wo'], w=[('pj', pi)])
                        dve(lambda e: e.tensor_tensor(out=x1[:, j, hf * 512:(hf + 1) * 512], in0=p_[:, :],
                                                      in1=xg[:, j, hf * 512:(hf + 1) * 512], op=ALU.add),
                            r=[('pj', pi), ('xg', gb)], w=[('x1', gb, j)])
                    dma('sp', out_d[tk:tk + 128, :], x1[:, j, :], r=[('x1', gb, j)], w=[('out_d', g * 4 + j)])
                    act(junk[:], x1[:, j, :], AF.Square, r=[('x1', gb, j)], w=['junk', ('s2a', gb, j)], accum_out=s2[:, j:j + 1])
                act(s2[:, 4:8], s2[:, 0:4], AF.Sqrt, r=[('s2a', gb, j) for j in range(4)], w=[('s2b', gb)], scale=1.0 / D, bias=EPS)
                dve(lambda e: e.reciprocal(out=s2[:, 8:12], in_=s2[:, 4:8]), r=[('s2b', gb)], w=[('s2c', gb)])

            outproj(0)
            for g in range(NG):
                t0 = g * 512
                gb = g % 2
                x1, s2 = x1l[gb], s2l[gb]
                if g + 1 < NG:
                    outproj(g + 1)
                for j in range(4):
                    xf = xn2f[j % 2]
                    XF = ('xn2f', j % 2)
                    dve(lambda e: e.scalar_tensor_tensor(out=xf[:], in0=x1[:, j, :], scalar=s2[:, 8 + j:9 + j], in1=gf[:],
                                                         op0=ALU.mult, op1=ALU.mult), r=[('x1', gb, j), ('s2c', gb), 'gf'], w=[XF])
                    act(xn2b[:, j, :], xf[:], AF.Copy, r=[XF], w=[('xn2b', j)])
                    for c in range(8):
                        tr(ptf[c // 4][:, (c % 4) * 128:(c % 4 + 1) * 128], xf[:, c * 128:(c + 1) * 128], cF('ident'),
                           r=[XF, 'cf'], w=[('ptf', c // 4)])
                    for hf in range(2):
                        act(xT[j % 2][:, hf * 4:(hf + 1) * 4, :], ptf[hf][:, :].rearrange("p (c t) -> p c t", t=128), AF.Copy,
                            r=[('ptf', hf)], w=[('xT', j % 2)])
                    for c in range(8):
                        mm(plg[:, j, 0:36], xT[j % 2][:, c, :], wr[:, c, :], c == 0, c == 7, r=[('xT', j % 2), 'wr'], w=['plg'])
                dve(lambda e: e.tensor_tensor(out=lg[:], in0=plg[:, :, 0:36], in1=br[:, None, :].to_broadcast([128, 4, 36]),
                                              op=ALU.add), r=['plg', 'br'], w=['lg'])
                dve(lambda e: e.tensor_reduce(out=gmx[:], in_=lg[:, :, 0:4], axis=AX.X, op=ALU.max), r=['lg'], w=['gmx'])
                dve(lambda e: e.tensor_tensor(out=goh[:], in0=lg[:, :, 0:4], in1=gmx[:, :, None].to_broadcast([128, 4, 4]),
                                              op=ALU.is_equal), r=['lg', 'gmx'], w=['goh'])
                dve(lambda e: e.tensor_tensor(out=gsub[:], in0=lg[:, :, 0:4], in1=gmx[:, :, None].to_broadcast([128, 4, 4]),
                                              op=ALU.subtract), r=['lg', 'gmx'], w=['gsub'])
                act(ge[:], gsub[:], AF.Exp, r=['gsub'], w=['ge'])
                dve(lambda e: e.tensor_reduce(out=gsum[:], in_=ge[:], axis=AX.X, op=ALU.add), r=['ge'], w=['gsum'])
                dve(lambda e: e.reciprocal(out=ggate[:], in_=gsum[:]), r=['gsum'], w=['ggate'])
                dve(lambda e: e.tensor_scalar(out=pen[:], in0=goh[:], scalar1=BIG, scalar2=-BIG, op0=ALU.mult, op1=ALU.add),
                    r=['goh'], w=['pen'])
                dve(lambda e: e.tensor_tensor(out=em[:].rearrange("p j (g e) -> p j g e", e=8),
                                              in0=lg[:, :, 4:36].rearrange("p j (g e) -> p j g e", e=8),
                                              in1=pen[:, :, :, None].to_broadcast([128, 4, 4, 8]), op=ALU.add),
                    r=['lg', 'pen'], w=['em'])
                dve(lambda e: e.tensor_reduce(out=m1[:], in_=em[:], axis=AX.X, op=ALU.max), r=['em'], w=['m1'])
                dve(lambda e: e.tensor_tensor(out=oh1[:], in0=em[:], in1=m1[:, :, None].to_broadcast([128, 4, 32]),
                                              op=ALU.is_equal), r=['em', 'm1'], w=['oh1'])
                dve(lambda e: e.scalar_tensor_tensor(out=em2[:], in0=oh1[:], scalar=-BIG, in1=em[:], op0=ALU.mult, op1=ALU.add),
                    r=['oh1', 'em'], w=['em2'])
                dve(lambda e: e.tensor_reduce(out=m2[:], in_=em2[:], axis=AX.X, op=ALU.max), r=['em2'], w=['m2'])
                dve(lambda e: e.tensor_tensor(out=oh2[:], in0=em2[:], in1=m2[:, :, None].to_broadcast([128, 4, 32]),
                                              op=ALU.is_equal), r=['em2', 'm2'], w=['oh2'])
                dve(lambda e: e.tensor_tensor(out=dm[:], in0=m2[:], in1=m1[:], op=ALU.subtract), r=['m1', 'm2'], w=['dm'])
                act(ex[:], dm[:], AF.Exp, r=['dm'], w=['ex'])
                dve(lambda e: e.tensor_scalar(out=p1[:], in0=ex[:], scalar1=1.0, scalar2=None, op0=ALU.add), r=['ex'], w=['p1'])
                dve(lambda e: e.reciprocal(out=p1[:], in_=p1[:]), r=['p1'], w=['p1'])
                dve(lambda e: e.tensor_tensor(out=RT[:, g * 4:g * 4 + 4, 0], in0=p1[:], in1=ggate[:], op=ALU.mult),
                    r=['p1', 'ggate'], w=['RT'])
                dve(lambda e: e.tensor_tensor(out=RT[:, g * 4:g * 4 + 4, 1], in0=RT[:, g * 4:g * 4 + 4, 0], in1=ex[:], op=ALU.mult),
                    r=['ex', 'RT'], w=['RT'])
                dve(lambda e: e.tensor_tensor(out=aa[:], in0=oh1[:], in1=oh2[:], op=ALU.add), r=['oh1', 'oh2'], w=['aa'])
                aaf = aa[:].rearrange("p j e -> p (j e)")
                mm(ppos[:, 0:128], cF('ustrict'), aaf, True, True, r=['cf', 'aa'], w=['ppos'])
                mm(ppos[:, 128:256], cF('ones'), aaf, True, True, r=['cf', 'aa'], w=['ppos'])
                for j in range(4):
                    dve(lambda e: e.tensor_tensor(out=slot[:, j, :], in0=ppos[:, j * 32:(j + 1) * 32], in1=carry[:], op=ALU.add),
                        r=['ppos', 'carry'], w=[('slot', j)])
                    dve(lambda e: e.tensor_tensor(out=carry[:], in0=ppos[:, 128 + j * 32:128 + (j + 1) * 32], in1=carry[:],
                                                  op=ALU.add), r=['ppos', 'carry'], w=['carry'])
                SL = [('slot', j) for j in range(4)]
                dve(lambda e: e.tensor_scalar(out=slot[:], in0=slot[:], scalar1=float(CAP - 1), scalar2=None, op0=ALU.min),
                    r=SL, w=SL)
                dve(lambda e: e.tensor_tensor(out=slot[:], in0=slot[:], in1=ebase[:, None, :].to_broadcast([128, 4, 32]), op=ALU.add),
                    r=SL + ['ebase'], w=SL)
                for k_, oh in enumerate((oh1, oh2)):
                    dve(lambda e: e.tensor_tensor(out=tmp[:], in0=slot[:], in1=oh[:], op=ALU.mult),
                        r=SL + ['oh1', 'oh2'], w=['tmp'])
                    dve(lambda e: e.tensor_reduce(out=dstf[:, :, k_], in_=tmp[:], axis=AX.X, op=ALU.add), r=['tmp'], w=['dstf'])
                dve(lambda e: e.tensor_copy(out=RTi[:, g * 4:g * 4 + 4, :], in_=dstf[:]), r=['dstf'], w=['RTi'])
                for j in range(4):
                    for k_ in range(2):
                        sc.dma('pool', lambda e: e.indirect_dma_start(
                            out=xs_d[:, :], out_offset=bass.IndirectOffsetOnAxis(ap=RTi[:, g * 4 + j, k_:k_ + 1], axis=0),
                            in_=xn2b[:, j, :], in_offset=None), r=['RTi', ('xn2b', j)], w=[uq()])

    def phase_F(l):
        GS = 384
        NGR = CAP // GS
        NB = GS // 128
        with ExitStack() as st:
            def sbt(name, shape, dt):
                return st.enter_context(nc.sbuf_tensor(f"F{l}_{name}", list(shape), dt))

            def pst(name, shape, dt=F32):
                return st.enter_context(nc.psum_tensor(f"F{l}_{name}", list(shape), dt))
            w1b = [sbt(f"w1b{i}", [128, 8, DEXP], BF16) for i in range(2)]
            w3b = [sbt(f"w3b{i}", [128, 8, DEXP], BF16) for i in range(2)]
            w2b = [sbt(f"w2b{i}", [128, 4, D], BF16) for i in range(2)]
            xsg = [sbt(f"xsg{i}", [128, NB, D], BF16) for i in range(2)]
            xT = [sbt(f"xT{i}", [128, 8, GS], BF16) for i in range(2)]
            s1 = [sbt(f"s1{i}", [128, GS], F32) for i in range(2)]
            hT = [sbt(f"hT{i}", [128, 4, GS], BF16) for i in range(2)]
            yb = [sbt(f"yb{i}", [128, D], BF16) for i in range(4)]
            ptx = [pst(f"ptx{i}", [128, 8, 128], BF16) for i in range(2)]
            ph1 = [pst(f"ph1{i}", [128, 512]) for i in range(2)]
            ph3 = [pst(f"ph3{i}", [128, 512]) for i in range(2)]
            pyy = [pst(f"pyy{i}", [128, 512]) for i in range(2)]
            nyb = [0]
            ntx = [0]
            nh = [0]
            gidx = 0
            groups = [(ex, gi) for ex in range(NEXP) for gi in range(NGR)]

            def load_w(ex):
                wi = ex % 2
                dma('pool', w1b[wi][:], w1_d[l, ex].rearrange("(kc p) n -> p kc n", p=128), r=[], w=[('w1b', wi)])
                dma('pool', w3b[wi][:], w3_d[l, ex].rearrange("(kc p) n -> p kc n", p=128), r=[], w=[('w3b', wi)])
                dma('pool', w2b[wi][:], w2_d[l, ex].rearrange("(kc p) n -> p kc n", p=128), r=[], w=[('w2b', wi)])

            def load_x(k):
                ex, gi = groups[k]
                r0 = ex * CAP + gi * GS
                dma('sp', xsg[k % 2][:], xs_d[r0:r0 + GS, :].rearrange("(b p) f -> p b f", p=128), r=[], w=[('xsg', k % 2)])

            load_w(0)
            load_x(0)
            for k, (ex, gi) in enumerate(groups):
                wi = ex % 2
                gb = k % 2
                r0 = ex * CAP + gi * GS
                if gi == 0 and ex + 1 < NEXP:
                    load_w(ex + 1)
                if k + 1 < len(groups):
                    load_x(k + 1)
                for b in range(NB):
                    ntx[0] += 1
                    ti = ntx[0] % 2
                    for c in range(8):
                        tr(ptx[ti][:, c, :], xsg[gb][:, b, c * 128:(c + 1) * 128], cB('ident'), r=[('xsg', gb), 'cb'], w=[('ptx', ti)])
                    if b % 2 == 0:
                        act(xT[gb][:, :, b * 128:(b + 1) * 128], ptx[ti][:, :, :], AF.Copy, r=[('ptx', ti)], w=[('xT', gb)])
                    else:
                        sc.op('dve', lambda e: e.tensor_copy(out=xT[gb][:, :, b * 128:(b + 1) * 128], in_=ptx[ti][:, :, :]),
                              r=[('ptx', ti)], w=[('xT', gb)])
                for m_ in range(4):
                    nh[0] += 1
                    hi = nh[0] % 2
                    for kc in range(8):
                        mm(ph1[hi][:, 0:GS], w1b[wi][:, kc, m_ * 128:(m_ + 1) * 128], xT[gb][:, kc, :], kc == 0, kc == 7,
                           r=[('w1b', wi), ('xT', gb)], w=[('ph1', hi)])
                    for kc in range(8):
                        mm(ph3[hi][:, 0:GS], w3b[wi][:, kc, m_ * 128:(m_ + 1) * 128], xT[gb][:, kc, :], kc == 0, kc == 7,
                           r=[('w3b', wi), ('xT', gb)], w=[('ph3', hi)])
                    act(s1[hi][:], ph1[hi][:, 0:GS], AF.Silu, r=[('ph1', hi)], w=[('s1', hi)])
                    sc.op('dve', lambda e: e.tensor_tensor(out=hT[gb][:, m_, :], in0=ph3[hi][:, 0:GS], in1=s1[hi][:], op=ALU.mult),
                          r=[('ph3', hi), ('s1', hi)], w=[('hT', gb, m_)])
                for b in range(NB):
                    nyb[0] += 1
                    yi = nyb[0] % 4
                    for hf in range(2):
                        for m_ in range(4):
                            mm(pyy[hf][:, :], hT[gb][:, m_, b * 128:(b + 1) * 128], w2b[wi][:, m_, hf * 512:(hf + 1) * 512],
                               m_ == 0, m_ == 3, r=[('hT', gb, m_), ('w2b', wi)], w=[('pyy', hf)])
                        if hf == 0:
                            act(yb[yi][:, 0:512], pyy[hf][:, :], AF.Copy, r=[('pyy', hf)], w=[('yb', yi)])
                        else:
                            sc.op('dve', lambda e: e.tensor_copy(out=yb[yi][:, 512:1024], in_=pyy[hf][:, :]),
                                  r=[('pyy', hf)], w=[('yb', yi)])
                    dma('sp', ys_d[r0 + b * 128:r0 + (b + 1) * 128, :], yb[yi][:], r=[('yb', yi)], w=[uq()])

    def phase_G(l):
        with ExitStack() as st:
            def sbt(name, shape, dt):
                return st.enter_context(nc.sbuf_tensor(f"G{l}_{name}", list(shape), dt))
            r1 = [sbt(f"r1{i}", [128, D], BF16) for i in range(4)]
            r2 = [sbt(f"r2{i}", [128, D], BF16) for i in range(4)]
            xx = [sbt(f"xx{i}", [128, D], F32) for i in range(4)]
            for ti in range(NT):
                i = ti % 4
                tk = ti * 128
                sc.dma('pool', lambda e: e.indirect_dma_start(
                    out=r1[i][:, :], out_offset=None, in_=ys_d[:, :],
                    in_offset=bass.IndirectOffsetOnAxis(ap=RTi[:, ti, 0:1], axis=0)), r=['RTi'], w=[('r1', i)])
                sc.dma('pool', lambda e: e.indirect_dma_start(
                    out=r2[i][:, :], out_offset=None, in_=ys_d[:, :],
                    in_offset=bass.IndirectOffsetOnAxis(ap=RTi[:, ti, 1:2], axis=0)), r=['RTi'], w=[('r2', i)])
                dma('act', xx[i][:], out_d[tk:tk + 128, :], r=[], w=[('xx', i)])
                sc.op('dve', lambda e: e.scalar_tensor_tensor(out=xx[i][:], in0=r1[i][:], scalar=RT[:, ti, 0:1], in1=xx[i][:],
                                                              op0=ALU.mult, op1=ALU.add), r=[('r1', i), 'RT', ('xx', i)], w=[('xx', i)])
                sc.op('dve', lambda e: e.scalar_tensor_tensor(out=xx[i][:], in0=r2[i][:], scalar=RT[:, ti, 1:2], in1=xx[i][:],
                                                              op0=ALU.mult, op1=ALU.add), r=[('r2', i), 'RT', ('xx', i)], w=[('xx', i)])
                dma('sp', out_d[tk:tk + 128, :], xx[i][:], r=[('xx', i)], w=[('out_d', ti)])

    xin = x_in
    for l in range(L):
        if 'A' in phases:
            phase_A(l, xin, fuse_g=(l > 0 and 'G' in phases))
            sc.barrier()
        if 'B' in phases:
            if l == 0 and 'D' in phases:
                sc.op('pool', lambda e: e.memset(zt[:], 0.0), w=['zt'])
                sc._wait('act', [sc.lastw['zt']])
                rows = NEXP * CAP
                bg_jobs.extend(
                    (lambda e, r0=r0: e.dma_start(out=xs_d[r0:r0 + 512, :].rearrange("(p k) f -> p (k f)", k=4), in_=zt[:]))
                    for r0 in range(0, rows, 512))
            phase_B(l)
            if bg_jobs:
                sc.dma_bulk('act', list(bg_jobs))
                del bg_jobs[:]
            sc.barrier()
        if 'C' in phases:
            phase_C(l)
            sc.barrier()
        if 'D' in phases:
            phase_D(l, xin)
            sc.barrier()
        if 'F' in phases:
            phase_F(l)
            sc.barrier()
        if 'G' in phases and l == L - 1:
            phase_G(l)
            sc.barrier()
        xin = out_d

    final = ['out_d', 'qk_att_d', 'qk_ml_d', 'va_d', 'vm_d', 'og_d', 'gates_d', 'yT_d']
    sc.finish(final)
    es.close()
    return nc, sc


def host_layout(inp, L, CAP=768):
    f = lambda a: np.ascontiguousarray(np.asarray(a, dtype=np.float32))
    m = {}
    m['consts'] = host_consts()
    m['ebase'] = np.tile((np.arange(32, dtype=np.float32) * CAP)[None, :], (128, 1))
    m['g_mix_fm'] = f(inp['g_norm_mix'][:L].reshape(L, 8, 128).transpose(0, 2, 1))
    m['w_in'] = f(inp['w_in'][:L])
    m['b_if'] = f(np.concatenate([inp['b_igate'][:L], inp['b_fgate'][:L]], axis=1))
    m['conv_fm'] = f(inp['conv_w'][:L].reshape(L, 4, 8, 128).transpose(0, 3, 2, 1).reshape(L, 128, 32))
    gq = np.tile(inp['g_q'][:L], (1, 2))[:, :, None]
    gk = np.tile(inp['g_k'][:L], (1, 2))[:, :, None]
    m['gqk2'] = f(np.concatenate([gq, gk], axis=2))
    m['w_out'] = f(inp['w_out'][:L])
    m['g_ffn'] = f(inp['g_norm_ffn'][:L])
    m['w_r'] = f(np.concatenate([inp['w_group'][:L], inp['w_expert_router'][:L]], axis=2))
    m['b_r'] = f(np.concatenate([inp['b_group'][:L], inp['b_expert_router'][:L]], axis=1))
    m['w1'] = f(inp['w1'][:L])
    m['w3'] = f(inp['w3'][:L])
    m['w2'] = f(inp['w2'][:L])
    return m


SEQ = 8192
DEPTH = 4
BATCH = 4
CAPACITY = 768


def kernel(**inputs):
    inp = {k: np.asarray(v) for k, v in inputs.items()}
    B, S, _ = inp['x'].shape
    L = inp['w_in'].shape[0]
    nc, _sc = build(S, L, dbg=False, CAP=CAPACITY)
    shared = host_layout(inp, L, CAPACITY)
    in_maps = []
    for b in range(B):
        m = dict(shared)
        m['x'] = np.ascontiguousarray(inp['x'][b], dtype=np.float32)
        in_maps.append(m)
    res = run_bass_kernel_spmd(nc, in_maps, core_ids=list(range(B)))
    out = np.stack([np.asarray(r['out'], dtype=np.float32) for r in res.results], axis=0)
    return out
```
